# Optimizing a Trainium2 kernel written in Bass

```python
import math
import jax, jax.numpy as jnp
from jax import lax
import numpy as np

D_MODEL = 1024
BATCH = 8
SEQ = 2048
DEPTH = 4

N_MLA_HEADS = 8
QK_NOPE_DIM = 64
QK_ROPE_DIM = 32
QK_HEAD_DIM = QK_NOPE_DIM + QK_ROPE_DIM
V_HEAD_DIM = 64
Q_LORA_RANK = 256
KV_LORA_RANK = 256
ROPE_THETA = 10000.0
Q_BLOCK = 128
LRU_WIDTH = D_MODEL
N_LRU_BLOCKS = 8
LRU_BLOCK = LRU_WIDTH // N_LRU_BLOCKS
CONV_WIDTH = 4
CONV_LEFT = 2
RG_LRU_C = 8.0
A_MIN_RAD = 0.9
A_MAX_RAD = 0.999
D_FF = 2816
N_EXPERTS = 8
TOP_K = 2
N_DENSE = (DEPTH + 1) // 2
N_MOE = DEPTH // 2
EPS = 1e-6

IN_SIZES = (Q_LORA_RANK, KV_LORA_RANK, QK_ROPE_DIM, LRU_WIDTH, LRU_WIDTH, 2 * D_MODEL)
IN_COLS = sum(IN_SIZES)
IN_SPLITS = [int(v) for v in np.cumsum(IN_SIZES)[:-1]]

kernel_name = "hybrid_rglru_mla_moe_adaln_encoder"


def rms_norm(x, g):
    x32 = x.astype(jnp.float32)
    y = x32 * lax.rsqrt(jnp.mean(x32 * x32, axis=-1, keepdims=True) + EPS)
    return (y * g.astype(jnp.float32)).astype(x.dtype)


def rope_tables(positions, dtype):
    inv_freq = 1.0 / (ROPE_THETA ** (jnp.arange(0, QK_ROPE_DIM, 2, dtype=jnp.float32) / QK_ROPE_DIM))
    ang = positions.astype(jnp.float32)[..., None] * inv_freq
    return jnp.cos(ang).astype(dtype), jnp.sin(ang).astype(dtype)


def rope(x, cos, sin):
    x1, x2 = jnp.split(x, 2, axis=-1)
    return jnp.concatenate([x1 * cos - x2 * sin, x1 * sin + x2 * cos], axis=-1)


def block_attention(q, k, v):
    B, S, H, Dh = q.shape
    scale = Dh ** -0.5
    kh = k.transpose(0, 2, 1, 3)
    vh = v.transpose(0, 2, 1, 3)
    qb = q.reshape(B, S // Q_BLOCK, Q_BLOCK, H, Dh).transpose(1, 0, 3, 2, 4)

    def one_block(q_blk):
        s = jnp.einsum('bhqd,bhkd->bhqk', q_blk, kh).astype(jnp.float32) * scale
        p = jax.nn.softmax(s, axis=-1)
        return jnp.einsum('bhqk,bhkd->bhqd', p.astype(vh.dtype), vh)

    o = lax.map(one_block, qb)
    return o.transpose(1, 0, 3, 2, 4).reshape(B, S, H, -1)


def mla_branch(cq, ckv, kr, cos, sin, q_norm_g, kv_norm_g, w_uq, w_ukv, q_head_g, k_head_g, w_o):
    B, S, _ = cq.shape
    q = (rms_norm(cq, q_norm_g) @ w_uq).reshape(B, S, N_MLA_HEADS, QK_HEAD_DIM)
    kv = (rms_norm(ckv, kv_norm_g) @ w_ukv).reshape(B, S, N_MLA_HEADS, QK_NOPE_DIM + V_HEAD_DIM)
    q_nope, q_pe = q[..., :QK_NOPE_DIM], q[..., QK_NOPE_DIM:]
    k_nope, v = kv[..., :QK_NOPE_DIM], kv[..., QK_NOPE_DIM:]
    q_pe = rope(q_pe, cos[:, :, None, :], sin[:, :, None, :])
    k_pe = rope(kr, cos, sin)
    q = jnp.concatenate([q_nope, q_pe], axis=-1)
    k = jnp.concatenate(
        [k_nope, jnp.broadcast_to(k_pe[:, :, None, :], (B, S, N_MLA_HEADS, QK_ROPE_DIM))], axis=-1)
    q = rms_norm(q, q_head_g)
    k = rms_norm(k, k_head_g)
    o = block_attention(q, k, v)
    return o.reshape(B, S, N_MLA_HEADS * V_HEAD_DIM) @ w_o


def lru_combine(left, right):
    a_l, b_l = left
    a_r, b_r = right
    return a_l * a_r, a_r * b_l + b_r


def rg_lru(u, w_gates, b_gates, a_param, reverse):
    B, S, W = u.shape
    g = jnp.einsum('bsnd,gnde->gbsne', u.reshape(B, S, N_LRU_BLOCKS, LRU_BLOCK), w_gates)
    g = (g.reshape(2, B, S, W) + b_gates[:, None, None, :]).astype(jnp.float32)
    r = jax.nn.sigmoid(g[0])
    i = jax.nn.sigmoid(g[1])
    log_a = -RG_LRU_C * r * jax.nn.softplus(a_param.astype(jnp.float32))
    a = jnp.exp(log_a)
    mult = jnp.sqrt(-jnp.expm1(2.0 * log_a))
    first = jnp.arange(S) == (S - 1 if reverse else 0)
    mult = jnp.where(first[None, :, None], 1.0, mult)
    b = mult * i * u.astype(jnp.float32)
    _, h = lax.associative_scan(lru_combine, (a, b), axis=1, reverse=reverse)
    return h.astype(u.dtype)


def lru_branch(u, y_gate, conv_w, conv_b, w_gates, b_gates, a_param, w_o):
    S = u.shape[1]
    up = jnp.pad(u, ((0, 0), (CONV_LEFT, CONV_WIDTH - 1 - CONV_LEFT), (0, 0)))
    uc = conv_b
    for tap in range(CONV_WIDTH):
        uc = uc + up[:, tap:tap + S] * conv_w[tap]
    h = rg_lru(uc, w_gates[0], b_gates[0], a_param[0], False) + \
        rg_lru(uc, w_gates[1], b_gates[1], a_param[1], True)
    return (h * jax.nn.gelu(y_gate)) @ w_o


def swiglu(h, w_gate, w_up, w_down):
    return (jax.nn.silu(h @ w_gate) * (h @ w_up)) @ w_down


def moe_ffn(h, w_router, w_gate, w_up, w_down):
    B, S, D = h.shape
    t = h.reshape(B * S, D)
    logits = (t @ w_router).astype(jnp.float32)
    top_v, top_i = lax.top_k(logits, TOP_K)
    top_w = jax.nn.softmax(top_v, axis=-1)
    combine = jnp.sum(jax.nn.one_hot(top_i, N_EXPERTS, dtype=jnp.float32) * top_w[..., None], axis=1)
    combine = combine.astype(t.dtype)
    out = jnp.zeros_like(t)
    for e in range(N_EXPERTS):
        out = out + combine[:, e:e + 1] * swiglu(t, w_gate[e], w_up[e], w_down[e])
    return out.reshape(B, S, D)


def setup_inputs(seed: int = 0) -> dict:
    key = jax.random.key(seed)
    ks = jax.random.split(key, 32)

    def nrm(k, shape, scale):
        return jax.random.normal(k, shape, jnp.float32) * scale

    def gain(k, shape):
        return 1.0 + 0.02 * jax.random.normal(k, shape, jnp.float32)

    L = DEPTH
    unif = jax.random.uniform(ks[20], (L, 2, LRU_WIDTH), jnp.float32, A_MIN_RAD ** 2, A_MAX_RAD ** 2)
    lru_a_param = jnp.log(jnp.expm1(-0.5 * jnp.log(unif)))
    return {
        "x": nrm(ks[0], (BATCH, SEQ, D_MODEL), 1.0),
        "c": nrm(ks[1], (BATCH, D_MODEL), 1.0),
        "positions": jnp.broadcast_to(jnp.arange(SEQ, dtype=jnp.int32), (BATCH, SEQ)),
        "ada_w": nrm(ks[2], (L, D_MODEL, 6 * D_MODEL), 0.5 * D_MODEL ** -0.5),
        "ada_b": nrm(ks[3], (L, 6 * D_MODEL), 0.02),
        "norm1_g": gain(ks[4], (L, D_MODEL)),
        "norm2_g": gain(ks[5], (L, D_MODEL)),
        "w_in": nrm(ks[6], (L, D_MODEL, IN_COLS), D_MODEL ** -0.5),
        "q_norm_g": gain(ks[7], (L, Q_LORA_RANK)),
        "kv_norm_g": gain(ks[8], (L, KV_LORA_RANK)),
        "w_uq": nrm(ks[9], (L, Q_LORA_RANK, N_MLA_HEADS * QK_HEAD_DIM), Q_LORA_RANK ** -0.5),
        "w_ukv": nrm(ks[10], (L, KV_LORA_RANK, N_MLA_HEADS * (QK_NOPE_DIM + V_HEAD_DIM)), KV_LORA_RANK ** -0.5),
        "q_head_g": gain(ks[11], (L, QK_HEAD_DIM)),
        "k_head_g": gain(ks[12], (L, QK_HEAD_DIM)),
        "w_o_mla": nrm(ks[13], (L, N_MLA_HEADS * V_HEAD_DIM, D_MODEL), (N_MLA_HEADS * V_HEAD_DIM) ** -0.5),
        "conv_w": nrm(ks[14], (L, CONV_WIDTH, LRU_WIDTH), CONV_WIDTH ** -0.5),
        "conv_b": nrm(ks[15], (L, LRU_WIDTH), 0.01),
        "lru_gate_w": nrm(ks[16], (L, 2, 2, N_LRU_BLOCKS, LRU_BLOCK, LRU_BLOCK), LRU_BLOCK ** -0.5),
        "lru_gate_b": nrm(ks[17], (L, 2, 2, LRU_WIDTH), 0.01),
        "lru_a_param": lru_a_param,
        "w_o_lru": nrm(ks[18], (L, LRU_WIDTH, D_MODEL), LRU_WIDTH ** -0.5),
        "w_out": nrm(ks[19], (L, D_MODEL, D_MODEL), D_MODEL ** -0.5),
        "ffn_w_gate": nrm(ks[21], (N_DENSE, D_MODEL, D_FF), D_MODEL ** -0.5),
        "ffn_w_up": nrm(ks[22], (N_DENSE, D_MODEL, D_FF), D_MODEL ** -0.5),
        "ffn_w_down": nrm(ks[23], (N_DENSE, D_FF, D_MODEL), D_FF ** -0.5),
        "moe_router": nrm(ks[24], (N_MOE, D_MODEL, N_EXPERTS), D_MODEL ** -0.5),
        "moe_w_gate": nrm(ks[25], (N_MOE, N_EXPERTS, D_MODEL, D_FF), D_MODEL ** -0.5),
        "moe_w_up": nrm(ks[26], (N_MOE, N_EXPERTS, D_MODEL, D_FF), D_MODEL ** -0.5),
        "moe_w_down": nrm(ks[27], (N_MOE, N_EXPERTS, D_FF, D_MODEL), D_FF ** -0.5),
    }


def reference(x, c, positions, ada_w, ada_b, norm1_g, norm2_g, w_in, q_norm_g, kv_norm_g,
              w_uq, w_ukv, q_head_g, k_head_g, w_o_mla, conv_w, conv_b, lru_gate_w, lru_gate_b,
              lru_a_param, w_o_lru, w_out, ffn_w_gate, ffn_w_up, ffn_w_down,
              moe_router, moe_w_gate, moe_w_up, moe_w_down):
    cos, sin = rope_tables(positions, x.dtype)
    c_act = jax.nn.silu(c)
    for l in range(DEPTH):
        mod = c_act @ ada_w[l] + ada_b[l]
        shift1, scale1, gate1, shift2, scale2, gate2 = [m[:, None, :] for m in jnp.split(mod, 6, axis=-1)]

        h = rms_norm(x, norm1_g[l]) * (1 + scale1) + shift1
        cq, ckv, kr, u, y_gate, g_branch = jnp.split(h @ w_in[l], IN_SPLITS, axis=-1)
        y_mla = mla_branch(cq, ckv, kr, cos, sin, q_norm_g[l], kv_norm_g[l], w_uq[l], w_ukv[l],
                           q_head_g[l], k_head_g[l], w_o_mla[l])
        y_lru = lru_branch(u, y_gate, conv_w[l], conv_b[l], lru_gate_w[l], lru_gate_b[l],
                           lru_a_param[l], w_o_lru[l])
        g_lru, g_mla = jnp.split(jax.nn.sigmoid(g_branch), 2, axis=-1)
        x = x + gate1 * ((g_lru * y_lru + g_mla * y_mla) @ w_out[l])

        h = rms_norm(x, norm2_g[l]) * (1 + scale2) + shift2
        if l % 2 == 0:
            f = swiglu(h, ffn_w_gate[l // 2], ffn_w_up[l // 2], ffn_w_down[l // 2])
        else:
            f = moe_ffn(h, moe_router[l // 2], moe_w_gate[l // 2], moe_w_up[l // 2], moe_w_down[l // 2])
        x = x + gate2 * f
    return x
```

```python
import math
from contextlib import ExitStack
import numpy as np
import concourse.bass as bass
import concourse.mybir as mybir
from concourse.bass_utils import run_bass_kernel_spmd

F32 = mybir.dt.float32; BF16 = mybir.dt.bfloat16; I32 = mybir.dt.int32
AF = mybir.ActivationFunctionType
ALU = mybir.AluOpType
AX = mybir.AxisListType

L_ALL = 4
D = 1024; T = 2048; NT = 16; NG = 4; KC = 8
DFF = 2816; NE = 8
EPS = 1e-6
IN_COLS = 4640
HSPLIT = [6, 6, 5, 5]


class G:
    __slots__ = ("name", "w", "r")

    def __init__(self, name):
        self.name = name; self.w = None; self.r = {}


class Q:
    def __init__(self, name, is_pe=False):
        self.name = name; self.is_pe = is_pe
        self.ops = []; self.seen = {}; self.cnt = 0; self.sem = None


class Prog:
    def __init__(self, nc, stack, dma_ring=12):
        self.nc = nc
        self.q = {}
        self.semobj = {}
        for n in ("pe", "act", "dve", "pool", "sp"):
            self.q[n] = Q(n, is_pe=(n == "pe"))
        for n in ("pe", "act", "dve", "pool"):
            self.q[n].sem = stack.enter_context(nc.semaphore("s_" + n))
            self.semobj[n] = self.q[n].sem
        self.rings = {}
        for rn in ("sp", "gq"):
            sems = [stack.enter_context(nc.semaphore("d_%s%d" % (rn, i))) for i in range(dma_ring)]
            self.rings[rn] = {"sems": sems, "uses": [0] * dma_ring, "pos": 0}
            for i, s in enumerate(sems):
                self.semobj["d_%s%d" % (rn, i)] = s

    def _waits(self, q, deps):
        need = {}
        for (k, v) in deps:
            if q.is_pe and k == "pe":
                continue
            if q.seen.get(k, 0) >= v:
                continue
            if need.get(k, 0) < v:
                need[k] = v
        for k, v in need.items():
            q.seen[k] = v
            sem = self.semobj[k]
            q.ops.append(lambda e, sem=sem, v=v: e.wait_ge(sem, v))

    @staticmethod
    def _deps(reads, writes):
        deps = []
        for g in reads:
            if g.w is not None:
                deps.append(g.w)
        for g in writes:
            if g.w is not None:
                deps.append(g.w)
            for k, v in g.r.items():
                deps.append((k, v))
        return deps

    @staticmethod
    def _mark(cid, reads, writes):
        k, v = cid
        for g in reads:
            if g.r.get(k, 0) < v:
                g.r[k] = v
        for g in writes:
            g.w = cid; g.r = {}

    def op(self, qn, fn, reads=(), writes=()):
        q = self.q[qn]
        self._waits(q, self._deps(reads, writes))
        q.cnt += 1
        sem = q.sem
        q.ops.append(lambda e, fn=fn, sem=sem: fn(e).then_inc(sem, 1))
        self._mark((qn, q.cnt), reads, writes)

    def dma(self, qn, out, in_, reads=(), writes=(), **kw):
        ring = self.rings[qn]
        q = self.q["sp"] if qn == "sp" else self.q["pool"]
        pos = ring["pos"]; ring["pos"] = (pos + 1) % len(ring["sems"])
        sem = ring["sems"][pos]; uses = ring["uses"][pos]; ring["uses"][pos] += 1
        key = "d_%s%d" % (qn, pos)
        deps = self._deps(reads, writes)
        if uses > 0:
            deps.append((key, 16 * uses))
        self._waits(q, deps)
        q.ops.append(lambda e, out=out, in_=in_, sem=sem, kw=kw: e.dma_start(out=out, in_=in_, **kw).then_inc(sem, 16))
        self._mark((key, 16 * (uses + 1)), reads, writes)

    def barrier(self):
        targets = []
        for n in ("pe", "act", "dve", "pool"):
            if self.q[n].cnt > 0:
                targets.append((n, self.q[n].cnt))
        for rn, ring in self.rings.items():
            for i, u in enumerate(ring["uses"]):
                if u > 0:
                    targets.append(("d_%s%d" % (rn, i), 16 * u))
        for n in ("pe", "act", "dve", "pool", "sp"):
            self._waits(self.q[n], [t for t in targets if t[0] != n])

    def emit(self, block):
        def mk(q):
            def body(e):
                for f in q.ops:
                    f(e)
            return body
        block.sync(mk(self.q["sp"]))
        block.tensor(mk(self.q["pe"]))
        block.scalar(mk(self.q["act"]))
        block.vector(mk(self.q["dve"]))
        block.gpsimd(mk(self.q["pool"]))


class Alloc:
    BASE = 16512
    TOP = 229344

    def __init__(self, nc):
        self.nc = nc; self.n = 0

    def at(self, name, shape, dt, off):
        sz = int(np.prod(shape[1:])) * (2 if dt == BF16 else 4)
        assert off % 4 == 0
        assert self.BASE + off + sz <= self.TOP, (name, off, sz)
        self.n += 1
        return self.nc.alloc_sbuf_tensor_at("%s_%d" % (name, self.n), list(shape), dt, offset=self.BASE + off), sz


class Region:
    def __init__(self, al, start, size, name):
        self.al = al; self.start = start; self.size = size; self.cur = 0; self.name = name

    def reset(self):
        self.cur = 0

    def alloc(self, name, shape, dt):
        cur = (self.cur + 63) // 64 * 64
        t, sz = self.al.at(name, shape, dt, self.start + cur)
        assert cur + sz <= self.size, ("region overflow", self.name, name, cur, sz, self.size)
        self.cur = cur + sz
        return t


def build_program(n_layers=L_ALL, debug=False, layers=None, nexp=NE):
    nc = bass.Bass("TRN2", target_bir_lowering=False)
    L = L_ALL

    def din(name, shape, dt=F32):
        return nc.dram_tensor(name, list(shape), dt, kind="ExternalInput").ap()

    x_d = din("x", [T, D]); c_d = din("c", [D]); pos_d = din("positions", [T], I32)
    ada_w = din("ada_w", [L, D, 6 * D]); ada_b = din("ada_b", [L, 6 * D])
    norm1_g = din("norm1_g", [L, D]); norm2_g = din("norm2_g", [L, D])
    w_in = din("w_in", [L, D, IN_COLS])
    q_norm_g = din("q_norm_g", [L, 256]); kv_norm_g = din("kv_norm_g", [L, 256])
    w_uq = din("w_uq", [L, 256, 768]); w_ukv = din("w_ukv", [L, 256, 1024])
    q_head_g = din("q_head_g", [L, 96]); k_head_g = din("k_head_g", [L, 96])
    w_o_mla = din("w_o_mla", [L, 512, D])
    conv_w = din("conv_w", [L, 4, D]); conv_b = din("conv_b", [L, D])
    lru_gate_w = din("lru_gate_w", [L, 2, 2, 8, 128, 128]); lru_gate_b = din("lru_gate_b", [L, 2, 2, D])
    lru_a_param = din("lru_a_param", [L, 2, D])
    w_o_lru = din("w_o_lru", [L, D, D]); w_out = din("w_out", [L, D, D])
    ffn_w_gate = din("ffn_w_gate", [2, D, DFF]); ffn_w_up = din("ffn_w_up", [2, D, DFF]); ffn_w_down = din("ffn_w_down", [2, DFF, D])
    moe_router = din("moe_router", [2, D, NE])
    moe_w_gate = din("moe_w_gate", [2, NE, D, DFF]); moe_w_up = din("moe_w_up", [2, NE, D, DFF]); moe_w_down = din("moe_w_down", [2, NE, DFF, D])
    out_d = nc.dram_tensor("out", [T, D], F32, kind="ExternalOutput").ap()
    x_scr = nc.dram_tensor("x_scr", [T, D], F32, kind="Internal").ap()
    ropeC_d = nc.dram_tensor("ropeC", [128, T], F32, kind="Internal").ap()
    ropeS_d = nc.dram_tensor("ropeS", [128, T], F32, kind="Internal").ap()
    dbg = {}
    if debug:
        for nm, shp in (("d_hT", [128, 8 * T]), ("d_oT", [64, 8 * T]), ("d_ylT", [128, 8 * T]), ("d_zT", [128, 8 * T]),
                        ("d_x1", [T, D]), ("d_cqn", [128, 2 * T]), ("d_qh", [96, T]), ("d_kh", [96, T]), ("d_comb", [128, 128])):
            dbg[nm] = nc.dram_tensor(nm, shp, F32, kind="ExternalOutput").ap()

    x_dt = x_d.rearrange("(t p) d -> p t d", p=128)
    xs_dt = x_scr.rearrange("(t p) d -> p t d", p=128)
    out_dt = out_d.rearrange("(t p) d -> p t d", p=128)

    with ExitStack() as st:
        P = Prog(nc, st)
        al = Alloc(nc)
        PERS = Region(al, 0, 16384, "pers")
        H = Region(al, 16384, 32768, "H")
        X = Region(al, 16384 + 32768, 65536, "X")
        W = Region(al, 16384 + 32768 + 65536, Alloc.TOP - Alloc.BASE - (16384 + 32768 + 65536), "W")

        banks = [st.enter_context(nc.psum_tensor("bank%d" % i, [128, 512], F32)) for i in range(8)]
        gb = [G("bank%d" % i) for i in range(8)]

        ident = PERS.alloc("ident", [128, 128], F32); g_ident = G("ident")
        ones_bf = PERS.alloc("ones_bf", [128, 128], BF16); g_ones_bf = G("ones_bf")
        ones_f = PERS.alloc("ones_f", [128, 64], F32); g_ones_f = G("ones_f")
        cact_bf = PERS.alloc("cact_bf", [128, 8], BF16); g_cact = G("cact")
        crep = PERS.alloc("crep", [128, 8, 128], BF16); g_crep = G("crep")
        gate_bc = [PERS.alloc("gate_bc%d" % i, [128, D], F32) for i in range(2)]; g_gate = [G("gate0"), G("gate1")]
        modT = PERS.alloc("modT", [128, 32], F32); g_modT = G("modT")
        sc = [PERS.alloc("sc%d" % i, [128, 8], F32) for i in range(2)]; g_sc = [G("sc0"), G("sc1")]
        parA = PERS.alloc("parA", [128, 68], F32); g_parA = G("parA")
        parB = PERS.alloc("parB", [128, 88], F32); g_parB = G("parB")
        qhg = PERS.alloc("qhg", [128, 1], F32); khg = PERS.alloc("khg", [128, 1], F32); g_hg = G("hg")
        nsp = PERS.alloc("nsp", [128, 16], F32); g_nsp = G("nsp")
        nsp2 = PERS.alloc("nsp2", [128, 16], F32)
        ss = PERS.alloc("ss", [128, 16], F32); g_ss = G("ss")
        rstd = PERS.alloc("rstd", [128, 16], F32); g_rstd = G("rstd")
        stageA = PERS.alloc("stageA", [128, 128], F32); g_stageA = G("stageA")
        stageB = PERS.alloc("stageB", [128, 128], F32); g_stageB = G("stageB")
        lg = PERS.alloc("lg", [128, 16, 8], F32); g_lg = G("lg")
        srt = PERS.alloc("srt", [128, 16, 8], F32); g_srt = G("srt")
        comb = PERS.alloc("comb", [128, 16, 8], F32); g_comb = G("comb")
        mask = PERS.alloc("mask", [128, 16, 8], F32); g_mask = G("mask")
        den = PERS.alloc("den", [128, 16], F32); g_den = G("den")
        cf = PERS.alloc("cf", [128, 8], F32); g_cf = G("cf")
        small_i = PERS.alloc("small_i", [128, 4], I32); small_f = PERS.alloc("small_f", [128, 4], F32); g_small = G("small")

        hT = H.alloc("hT", [128, 8, T], BF16)
        g_hT = [[G("hT%d_%d" % (c, g)) for g in range(NG)] for c in range(KC)]
        x_sb = X.alloc("x_sb", [128, NT, D], F32)
        g_x = [G("x%d" % t) for t in range(NT)]
        g_xscr = [G("xscr%d" % t) for t in range(NT)]
        g_rope_d = G("rope_d")

        rr = {"act_dve": 0}

        def alt(*names):
            rr["act_dve"] += 1
            return names[rr["act_dve"] % len(names)]

        P.op("pool", lambda e: e.iota(stageA[:].bitcast(I32), pattern=[[1, 128]], base=0, channel_multiplier=-1), writes=[g_stageA])
        P.op("dve", lambda e: e.tensor_single_scalar(out=ident[:], in_=stageA[:].bitcast(I32), scalar=0, op=ALU.is_equal), reads=[g_stageA], writes=[g_ident])
        P.op("dve", lambda e: e.memset(ones_bf[:], 1.0), writes=[g_ones_bf])
        P.op("dve", lambda e: e.memset(ones_f[:], 1.0), writes=[g_ones_f])
        P.dma("sp", stageB[0:8, :], c_d.rearrange("(k p) -> k p", p=128), writes=[g_stageB])
        P.op("pe", lambda e: e.transpose(banks[7][:, 0:8], stageB[0:8, :], ident[0:8, 0:8]), reads=[g_stageB, g_ident], writes=[gb[7]])
        P.op("act", lambda e: e.activation(out=cf[:], in_=banks[7][:, 0:8], func=AF.Silu), reads=[gb[7]], writes=[g_cf])
        P.op("dve", lambda e: e.tensor_copy(out=cact_bf[:], in_=cf[:]), reads=[g_cf], writes=[g_cact])
        for k in range(8):
            P.op("dve", lambda e, k=k: e.tensor_scalar(out=crep[:, k, :], in0=ones_bf[:], scalar1=cf[:, k:k + 1], scalar2=None, op0=ALU.mult),
                 reads=[g_cf, g_ones_bf], writes=[g_crep])

        W.reset()
        pos_i = W.alloc("pos_i", [128, T], I32); pos_f = W.alloc("pos_f", [128, T], F32)
        ang = W.alloc("ang", [128, T], F32); a2 = W.alloc("a2", [128, T], F32); tq = W.alloc("tq", [128, T], F32)
        ti = W.alloc("ti", [128, T], I32); rr_t = W.alloc("rr_t", [128, T], F32); mk = W.alloc("mk", [128, T], F32)
        tabs = [W.alloc("tabS", [128, T], F32), W.alloc("tabC", [128, T], F32)]
        g_r = G("ropework")
        P.dma("sp", pos_i[:], pos_d.partition_broadcast(128), writes=[g_r])
        P.op("dve", lambda e: e.tensor_copy(out=pos_f[:], in_=pos_i[:]), reads=[g_r], writes=[g_r])
        P.op("pool", lambda e: e.iota(small_i[:, 0:1], pattern=[[1, 1]], base=0, channel_multiplier=1), writes=[g_small])
        P.op("dve", lambda e: e.tensor_single_scalar(out=small_i[:, 1:2], in_=small_i[:, 0:1], scalar=15, op=ALU.bitwise_and), reads=[g_small], writes=[g_small])
        P.op("dve", lambda e: e.tensor_single_scalar(out=small_i[:, 2:3], in_=small_i[:, 0:1], scalar=16, op=ALU.bitwise_and), reads=[g_small], writes=[g_small])
        P.op("dve", lambda e: e.tensor_copy(out=small_f[:, 1:3], in_=small_i[:, 1:3]), reads=[g_small], writes=[g_small])
        P.op("act", lambda e: e.activation(out=small_f[:, 0:1], in_=small_f[:, 1:2], func=AF.Exp, scale=-math.log(10000.0) / 16.0), reads=[g_small], writes=[g_small])
        P.op("dve", lambda e: e.tensor_scalar(out=small_f[:, 3:4], in0=small_f[:, 2:3], scalar1=2.0 / 16.0, scalar2=-1.0, op0=ALU.mult, op1=ALU.add), reads=[g_small], writes=[g_small])
        P.op("dve", lambda e: e.tensor_scalar(out=ang[:], in0=pos_f[:], scalar1=small_f[:, 0:1], scalar2=None, op0=ALU.mult), reads=[g_r, g_small], writes=[g_r])
        C1 = 6.28125; C2 = 2 * math.pi - C1
        for ti_, shift in ((0, 0.0), (1, math.pi / 2)):
            tab = tabs[ti_]
            P.op("dve", lambda e, shift=shift: e.tensor_scalar(out=a2[:], in0=ang[:], scalar1=shift, scalar2=None, op0=ALU.add), reads=[g_r], writes=[g_r])
            P.op("dve", lambda e: e.tensor_scalar(out=tq[:], in0=a2[:], scalar1=1.0 / (2 * math.pi), scalar2=None, op0=ALU.mult), reads=[g_r], writes=[g_r])
            P.op("dve", lambda e: e.tensor_copy(out=ti[:], in_=tq[:]), reads=[g_r], writes=[g_r])
            P.op("dve", lambda e: e.tensor_copy(out=tq[:], in_=ti[:]), reads=[g_r], writes=[g_r])
            P.op("dve", lambda e: e.scalar_tensor_tensor(out=rr_t[:], in0=tq[:], scalar=-C1, in1=a2[:], op0=ALU.mult, op1=ALU.add), reads=[g_r], writes=[g_r])
            P.op("dve", lambda e: e.scalar_tensor_tensor(out=a2[:], in0=tq[:], scalar=-C2, in1=rr_t[:], op0=ALU.mult, op1=ALU.add), reads=[g_r], writes=[g_r])
            P.op("dve", lambda e: e.tensor_single_scalar(out=mk[:], in_=a2[:], scalar=math.pi, op=ALU.is_gt), reads=[g_r], writes=[g_r])
            P.op("dve", lambda e: e.scalar_tensor_tensor(out=rr_t[:], in0=mk[:], scalar=-2 * math.pi, in1=a2[:], op0=ALU.mult, op1=ALU.add), reads=[g_r], writes=[g_r])
            P.op("dve", lambda e: e.tensor_single_scalar(out=mk[:], in_=rr_t[:], scalar=-math.pi, op=ALU.is_lt), reads=[g_r], writes=[g_r])
            P.op("dve", lambda e: e.scalar_tensor_tensor(out=a2[:], in0=mk[:], scalar=2 * math.pi, in1=rr_t[:], op0=ALU.mult, op1=ALU.add), reads=[g_r], writes=[g_r])
            P.op("dve", lambda e: e.tensor_scalar(out=a2[:], in0=a2[:], scalar1=-3.14159, scalar2=3.14159, op0=ALU.max, op1=ALU.min), reads=[g_r], writes=[g_r])
            P.op("act", lambda e, tab=tab: e.activation(out=tab[:], in_=a2[:], func=AF.Sin), reads=[g_r], writes=[g_r])
        P.op("dve", lambda e: e.tensor_scalar(out=tabs[0][:], in0=tabs[0][:], scalar1=small_f[:, 3:4], scalar2=None, op0=ALU.mult), reads=[g_r, g_small], writes=[g_r])
        P.op("dve", lambda e: e.memset(tabs[0][0:64, :], 0.0), reads=[g_r], writes=[g_r])
        P.op("dve", lambda e: e.memset(tabs[1][0:64, :], 1.0), reads=[g_r], writes=[g_r])
        P.dma("sp", ropeS_d[:, :], tabs[0][:], reads=[g_r], writes=[g_rope_d])
        P.dma("sp", ropeC_d[:, :], tabs[1][:], reads=[g_r], writes=[g_rope_d])

        for t in range(NT):
            P.dma("sp", x_sb[:, t, :], x_dt[:, t, :], writes=[g_x[t]])
        P.barrier()

        def wview(w2d):
            return w2d.rearrange("(k p) n -> p k n", p=128)

        def mm_group(bank_i, rows, cols, pairs, reads, n0=0):
            n = len(pairs)
            for i, (lt, rh) in enumerate(pairs):
                P.op("pe", lambda e, lt=lt, rh=rh, i=i: e.matmul(banks[bank_i][0:rows, n0:n0 + cols], lhsT=lt, rhs=rh, start=(i == 0), stop=(i == n - 1)),
                     reads=reads, writes=[gb[bank_i]])

        def rsqrt_chain(dst, src_ap, src_g, dst_g, scale, rows=128):
            P.op("dve", lambda e: e.tensor_scalar(out=dst, in0=src_ap, scalar1=scale, scalar2=EPS, op0=ALU.mult, op1=ALU.add), reads=[src_g], writes=[dst_g])
            P.op("act", lambda e: e.activation(out=dst, in_=dst, func=AF.Ln), reads=[dst_g], writes=[dst_g])
            P.op("act", lambda e: e.activation(out=dst, in_=dst, func=AF.Exp, scale=-0.5), reads=[dst_g], writes=[dst_g])

        def load_params(l):
            P.dma("sp", stageA[0:48, :], ada_b[l].rearrange("(j p) -> j p", p=128), writes=[g_stageA])
            P.dma("sp", stageA[48:56, :], norm1_g[l].rearrange("(j p) -> j p", p=128), writes=[g_stageA])
            P.dma("sp", stageA[56:64, :], norm2_g[l].rearrange("(j p) -> j p", p=128), writes=[g_stageA])
            P.dma("sp", stageA[64:66, :], q_norm_g[l].rearrange("(j p) -> j p", p=128), writes=[g_stageA])
            P.dma("sp", stageA[66:68, :], kv_norm_g[l].rearrange("(j p) -> j p", p=128), writes=[g_stageA])
            P.dma("sp", stageB[0:32, :], conv_w[l].rearrange("t (j p) -> (t j) p", p=128), writes=[g_stageB])
            P.dma("sp", stageB[32:40, :], conv_b[l].rearrange("(j p) -> j p", p=128), writes=[g_stageB])
            P.dma("sp", stageB[40:72, :], lru_gate_b[l].rearrange("a b (j p) -> (a b j) p", p=128), writes=[g_stageB])
            P.dma("sp", stageB[72:88, :], lru_a_param[l].rearrange("a (j p) -> (a j) p", p=128), writes=[g_stageB])
            P.op("pe", lambda e: e.transpose(banks[7][:, 0:68], stageA[0:68, :], ident[0:68, 0:68]), reads=[g_stageA, g_ident], writes=[gb[7]])
            P.op("dve", lambda e: e.tensor_copy(out=parA[:], in_=banks[7][:, 0:68]), reads=[gb[7]], writes=[g_parA])
            P.op("pe", lambda e: e.transpose(banks[7][:, 0:88], stageB[0:88, :], ident[0:88, 0:88]), reads=[g_stageB, g_ident], writes=[gb[7]])
            P.op("dve", lambda e: e.tensor_copy(out=parB[:], in_=banks[7][:, 0:88]), reads=[gb[7]], writes=[g_parB])
            P.dma("sp", qhg[0:96, :], q_head_g[l].rearrange("(p o) -> p o", o=1), writes=[g_hg])
            P.dma("sp", khg[0:96, :], k_head_g[l].rearrange("(p o) -> p o", o=1), writes=[g_hg])
            P.op("dve", lambda e: e.tensor_scalar(out=qhg[0:96, :], in0=qhg[0:96, :], scalar1=96.0 ** -0.5, scalar2=None, op0=ALU.mult), reads=[g_hg], writes=[g_hg])
            P.op("act", lambda e: e.activation(out=nsp[:], in_=parB[:, 72:88], func=AF.Exp), reads=[g_parB], writes=[g_nsp])
            P.op("act", lambda e: e.activation(out=nsp[:], in_=nsp[:], func=AF.Ln, bias=1.0), reads=[g_nsp], writes=[g_nsp])
            P.op("dve", lambda e: e.tensor_scalar(out=nsp[:], in0=nsp[:], scalar1=-8.0, scalar2=None, op0=ALU.mult), reads=[g_nsp], writes=[g_nsp])
            P.op("dve", lambda e: e.tensor_scalar(out=nsp2[:], in0=nsp[:], scalar1=2.0, scalar2=None, op0=ALU.mult), reads=[g_nsp], writes=[g_nsp])

        ADA_FM = {0: 0, 1: 1, 3: 2, 4: 3}

        def ada_gate_bias(l, gi):
            s_ = 2 if gi == 0 else 5
            P.dma("sp", gate_bc[gi][:], ada_b[l, s_ * D:(s_ + 1) * D].partition_broadcast(128), writes=[g_gate[gi]])

        def ada_dma(l, b, wb, g_wb):
            i = b % 2
            P.dma("gq", wb[i][:], wview(ada_w[l])[:, :, b * 512:(b + 1) * 512], writes=[g_wb[i]])

        def ada_compute(l, b, wb, g_wb):
            s_ = b // 2; half = b % 2; i = b % 2
            if s_ in ADA_FM:
                m0 = ADA_FM[s_] * 8 + half * 4
                for jj in range(4):
                    mm_group(7, 128, 1, [(wb[i][:, k, jj * 128:(jj + 1) * 128], cact_bf[:, k:k + 1]) for k in range(8)], [g_wb[i], g_cact], n0=m0 + jj)
                pc = s_ * 8 + half * 4
                P.op("dve", lambda e: e.tensor_tensor(out=modT[:, m0:m0 + 4], in0=banks[7][:, m0:m0 + 4], in1=parA[:, pc:pc + 4], op=ALU.add),
                     reads=[gb[7], g_parA], writes=[g_modT])
            else:
                gi = 0 if s_ == 2 else 1
                bi = b % 2
                mm_group(bi, 128, 512, [(crep[:, k, :], wb[i][:, k, :]) for k in range(8)], [g_wb[i], g_crep])
                P.op("dve", lambda e: e.tensor_tensor(out=gate_bc[gi][:, half * 512:(half + 1) * 512], in0=banks[bi][:, 0:512],
                                                      in1=gate_bc[gi][:, half * 512:(half + 1) * 512], op=ALU.add),
                     reads=[gb[bi], g_gate[gi]], writes=[g_gate[gi]])

        def ada_final():
            P.op("dve", lambda e: e.scalar_tensor_tensor(out=sc[0][:], in0=modT[:, 8:16], scalar=1.0, in1=parA[:, 48:56], op0=ALU.add, op1=ALU.mult), reads=[g_modT, g_parA], writes=[g_sc[0]])
            P.op("dve", lambda e: e.scalar_tensor_tensor(out=sc[1][:], in0=modT[:, 24:32], scalar=1.0, in1=parA[:, 56:64], op0=ALU.add, op1=ALU.mult), reads=[g_modT, g_parA], writes=[g_sc[1]])

        def adaln(l):
            W.reset()
            wb = [W.alloc("adaw%d" % i, [128, 8, 512], BF16) for i in range(2)]
            g_wb = [G("adaw0"), G("adaw1")]
            ada_gate_bias(l, 0); ada_gate_bias(l, 1)
            for b in range(12):
                ada_dma(l, b, wb, g_wb)
                ada_compute(l, b, wb, g_wb)
            ada_final()

        def norm_phase(which, Wr, router_w=None):
            scv = sc[which]; shc = 0 if which == 0 else 16
            junk = Wr.alloc("junk", [128, D], BF16); g_junk = G("junk")
            xn = [Wr.alloc("xn%d" % i, [128, D], F32) for i in range(8)]; g_xn = [G("xn%d" % i) for i in range(8)]
            if router_w is not None:
                h2f = Wr.alloc("h2f", [128, 8, 512], F32); g_h2f = [G("h2f%d" % c) for c in range(8)]
                wr_f = Wr.alloc("wr_f", [128, 8, NE], F32); g_wr = G("wr_f")
                w_hi = Wr.alloc("w_hi", [128, 8, NE], BF16); w_lo = Wr.alloc("w_lo", [128, 8, NE], BF16)
                h_hi = Wr.alloc("h_hi", [128, 8, 512], BF16); h_lo = Wr.alloc("h_lo", [128, 8, 512], BF16)
                g_hhi = [G("hhi%d" % c) for c in range(8)]; g_hlo = [G("hlo%d" % c) for c in range(8)]
                for k_ in range(8):
                    P.dma("sp", wr_f[:, k_, :], router_w[k_ * 128:(k_ + 1) * 128, :], writes=[g_wr])
                P.op("dve", lambda e: e.tensor_copy(out=w_hi[:], in_=wr_f[:]), reads=[g_wr], writes=[g_wr])
                P.op("dve", lambda e: e.tensor_tensor(out=w_lo[:], in0=wr_f[:], in1=w_hi[:], op=ALU.subtract), reads=[g_wr], writes=[g_wr])
            for t in range(NT):
                P.op("act", lambda e, t=t: e.activation(out=junk[:], in_=x_sb[:, t, :], func=AF.Square, accum_out=ss[:, t:t + 1]), reads=[g_x[t]], writes=[g_junk, g_ss])
            rsqrt_chain(rstd[:], ss[:], g_ss, g_rstd, 1.0 / D)
            for g in range(NG):
                for tt in range(4):
                    t = 4 * g + tt; i = t % 8
                    P.op("dve", lambda e, t=t, i=i: e.tensor_scalar(out=xn[i][:], in0=x_sb[:, t, :], scalar1=rstd[:, t:t + 1], scalar2=None, op0=ALU.mult),
                         reads=[g_x[t], g_rstd], writes=[g_xn[i]])
                for c in range(KC):
                    bi = c % 4
                    for tt in range(4):
                        i = (4 * g + tt) % 8
                        P.op("pe", lambda e, bi=bi, tt=tt, i=i, c=c: e.transpose(banks[bi][:, tt * 128:(tt + 1) * 128], xn[i][:, c * 128:(c + 1) * 128], ident[:]),
                             reads=[g_xn[i], g_ident], writes=[gb[bi]])
                    P.op("act", lambda e, bi=bi, c=c, g=g: e.activation(out=hT[:, c, g * 512:(g + 1) * 512], in_=banks[bi][:, 0:512], func=AF.Identity,
                                                                         scale=scv[:, c:c + 1], bias=modT[:, shc + c:shc + c + 1]),
                         reads=[gb[bi], g_sc[which], g_modT], writes=[g_hT[c][g]])
                    if router_w is not None:
                        P.op("dve", lambda e, bi=bi, c=c: e.tensor_scalar(out=h2f[:, c, :], in0=banks[bi][:, 0:512], scalar1=scv[:, c:c + 1], scalar2=modT[:, shc + c:shc + c + 1],
                                                                          op0=ALU.mult, op1=ALU.add),
                             reads=[gb[bi], g_sc[which], g_modT, g_hT[c][g]], writes=[g_h2f[c]])
                        P.op("act", lambda e, c=c: e.activation(out=h_hi[:, c, :], in_=h2f[:, c, :], func=AF.Copy), reads=[g_h2f[c]], writes=[g_hhi[c]])
                        P.op("dve", lambda e, c=c: e.tensor_tensor(out=h_lo[:, c, :], in0=h2f[:, c, :], in1=h_hi[:, c, :], op=ALU.subtract), reads=[g_h2f[c], g_hhi[c]], writes=[g_hlo[c]])
                if router_w is not None:
                    for tt in range(4):
                        t = 4 * g + tt
                        prs = []
                        for c in range(8):
                            prs.append((h_hi[:, c, tt * 128:(tt + 1) * 128], w_hi[:, c, :]))
                            prs.append((h_lo[:, c, tt * 128:(tt + 1) * 128], w_hi[:, c, :]))
                            prs.append((h_hi[:, c, tt * 128:(tt + 1) * 128], w_lo[:, c, :]))
                        mm_group(7, 128, 8, prs, g_hhi + g_hlo + [g_wr], n0=t * 8)
            if router_w is not None:
                P.op("dve", lambda e: e.tensor_copy(out=lg[:].rearrange("p a b -> p (a b)"), in_=banks[7][:, 0:128]), reads=[gb[7]], writes=[g_lg])
                for t in range(NT):
                    P.op("dve", lambda e, t=t: e.max(out=srt[:, t, :], in_=lg[:, t, :]), reads=[g_lg], writes=[g_srt])
                P.op("dve", lambda e: e.tensor_tensor(out=mask[:], in0=lg[:], in1=srt[:, :, 1:2].to_broadcast([128, 16, 8]), op=ALU.is_ge), reads=[g_lg, g_srt], writes=[g_mask])
                P.op("dve", lambda e: e.tensor_tensor(out=comb[:], in0=lg[:], in1=srt[:, :, 0:1].to_broadcast([128, 16, 8]), op=ALU.subtract), reads=[g_lg, g_srt], writes=[g_comb])
                P.op("act", lambda e: e.activation(out=comb[:], in_=comb[:], func=AF.Exp), reads=[g_comb], writes=[g_comb])
                P.op("dve", lambda e: e.tensor_tensor(out=comb[:], in0=comb[:], in1=mask[:], op=ALU.mult), reads=[g_comb, g_mask], writes=[g_comb])
                P.op("dve", lambda e: e.tensor_reduce(out=den[:], in_=comb[:], op=ALU.add, axis=AX.X), reads=[g_comb], writes=[g_den])
                P.op("dve", lambda e: e.reciprocal(out=den[:], in_=den[:]), reads=[g_den], writes=[g_den])
                P.op("dve", lambda e: e.tensor_tensor(out=comb[:], in0=comb[:], in1=den[:].unsqueeze(2).to_broadcast([128, 16, 8]), op=ALU.mult), reads=[g_comb, g_den], writes=[g_comb])

        layers = list(range(n_layers)) if layers is None else layers
        for li_, l in enumerate(layers):
            l_next = layers[li_ + 1] if li_ + 1 < len(layers) else None
            if li_ == 0:
                load_params(l)
                adaln(l)
                P.barrier()
            W.reset()
            norm_phase(0, W)
            if debug and l == 0:
                P.dma("gq", dbg["d_hT"].rearrange("p (c t) -> p c t", c=8), hT[:], reads=[g for row in g_hT for g in row])
            P.barrier()
            hT_all = [g for row in g_hT for g in row]

            X.reset(); W.reset()
            oT = X.alloc("oT", [64, 8, T], BF16); g_oT = [G("oT%d" % h) for h in range(8)]
            ropeC = X.alloc("ropeC", [128, T], F32); ropeS = X.alloc("ropeS", [128, T], F32); g_rope = G("rope")
            kpe = X.alloc("kpe", [128, T], F32); g_kpe = G("kpe")
            qh = [X.alloc("qh0", [128, T], BF16), W.alloc("qh1", [128, T], BF16)]; g_qh = [G("qh0"), G("qh1")]
            kh = [X.alloc("kh0", [128, T], BF16), W.alloc("kh1", [128, T], BF16)]; g_kh = [G("kh0"), G("kh1")]
            cqn = W.alloc("cqn", [128, 2, T], BF16); ckvn = W.alloc("ckvn", [128, 2, T], BF16)
            g_cqn = [G("cqn%d" % g) for g in range(NG)]; g_ckvn = [G("ckvn%d" % g) for g in range(NG)]
            V_sb = W.alloc("V_sb", [128, NT, 8, 65], BF16); g_V = [G("V%d" % t) for t in range(NT)]
            PT = [W.alloc("PT%d" % i, [128, 512], BF16) for i in range(4)]; g_PT = [G("PT%d" % i) for i in range(4)]
            in01 = W.alloc("in01", [128, 8, 512], BF16); g_in01 = G("in01")
            wkr = W.alloc("wkr", [128, 8, 96], BF16); wkr_sw = W.alloc("wkr_sw", [128, 8, 96], BF16); g_wkr = G("wkr"); g_wkrsw = G("wkrsw")
            wq = W.alloc("wq", [128, 2, 768], BF16); wq_sw = W.alloc("wq_sw", [128, 2, 8, 96], BF16); g_wq = G("wq"); g_wqsw = G("wqsw")
            wkn = W.alloc("wkn", [128, 2, 8, 64], BF16); wv = W.alloc("wv", [128, 2, 8, 64], BF16); g_wkn = G("wkn"); g_wv = G("wv")
            sqb = [W.alloc("sqb%d" % i, [128, 512], BF16) for i in range(2)]; g_sqb = [G("sqb0"), G("sqb1")]
            f1 = W.alloc("f1", [128, 512], F32); f2 = W.alloc("f2", [128, 512], F32); f3 = W.alloc("f3", [128, 512], F32); rs = W.alloc("rs", [128, 512], F32)
            g_f1, g_f2, g_f3, g_rs = G("f1"), G("f2"), G("f3"), G("rs")
            rden = W.alloc("rden", [128, 512], F32); b_sb = W.alloc("b_sb", [64, 512], F32); g_rden = G("rden"); g_bsb = G("bsb")

            P.dma("sp", ropeC[:], ropeC_d[:, :], reads=[g_rope_d], writes=[g_rope])
            P.dma("sp", ropeS[:], ropeS_d[:, :], reads=[g_rope_d], writes=[g_rope])
            wi = wview(w_in[l])
            P.dma("gq", in01[:], wi[:, :, 0:512], writes=[g_in01])
            P.dma("gq", wkr[:], wi[:, :, 448:544], writes=[g_wkr])
            P.op("dve", lambda e: e.memset(wkr_sw[:], 0.0), writes=[g_wkrsw])
            P.dma("gq", wkr_sw[:, :, 64:80], wi[:, :, 528:544], writes=[g_wkrsw])
            P.dma("gq", wkr_sw[:, :, 80:96], wi[:, :, 512:528], writes=[g_wkrsw])
            wuq = w_uq[l].rearrange("(k p) n -> p k n", p=128)
            P.dma("gq", wq[:], wuq, writes=[g_wq])
            P.op("dve", lambda e: e.memset(wq_sw[:], 0.0), writes=[g_wqsw])
            wuq4 = w_uq[l].rearrange("(k p) (h e) -> p k h e", p=128, e=96)
            for k in range(2):
                P.dma("gq", wq_sw[:, k, :, 64:80], wuq4[:, k, :, 80:96], writes=[g_wqsw])
                P.dma("gq", wq_sw[:, k, :, 80:96], wuq4[:, k, :, 64:80], writes=[g_wqsw])
            wukv4 = w_ukv[l].rearrange("(k p) (h e) -> p k h e", p=128, e=128)
            for k in range(2):
                P.dma("gq", wkn[:, k], wukv4[:, k, :, 0:64], writes=[g_wkn])
                P.dma("gq", wv[:, k], wukv4[:, k, :, 64:128], writes=[g_wv])
            P.op("dve", lambda e: e.memset(V_sb[:, :, :, 64:65], 1.0), writes=g_V)

            for g in range(NG):
                gs = slice(g * 512, (g + 1) * 512)
                hT_g = [g_hT[c][g] for c in range(KC)]
                for (dst, g_dst, off, pcol) in ((cqn, g_cqn, 0, 64), (ckvn, g_ckvn, 256, 66)):
                    for ci in range(2):
                        mm_group(ci, 128, 512, [(in01[:, k, off + ci * 128: off + (ci + 1) * 128], hT[:, k, gs]) for k in range(KC)], hT_g + [g_in01])
                        P.op("act", lambda e, ci=ci: e.activation(out=sqb[ci][:], in_=banks[ci][:, :], func=AF.Square), reads=[gb[ci]], writes=[g_sqb[ci]])
                    mm_group(4, 128, 512, [(ones_bf[:], sqb[0][:]), (ones_bf[:], sqb[1][:])], [g_ones_bf] + g_sqb)
                    rsqrt_chain(rs[:], banks[4][:, :], gb[4], g_rs, 1.0 / 256)
                    for ci in range(2):
                        P.op("dve", lambda e, ci=ci, dst=dst, pcol=pcol, gs=gs: e.scalar_tensor_tensor(out=dst[:, ci, gs], in0=banks[ci][:, :], scalar=parA[:, pcol + ci:pcol + ci + 1],
                                                                                                 in1=rs[:], op0=ALU.mult, op1=ALU.mult),
                             reads=[gb[ci], g_parA, g_rs], writes=[g_dst[g]])
                mm_group(2, 96, 512, [(wkr[:, k, :], hT[:, k, gs]) for k in range(KC)], hT_g + [g_wkr])
                mm_group(3, 96, 512, [(wkr_sw[:, k, :], hT[:, k, gs]) for k in range(KC)], hT_g + [g_wkrsw])
                P.op("dve", lambda e, gs=gs: e.tensor_tensor(out=f1[64:96, :], in0=banks[2][64:96, :], in1=ropeC[64:96, gs], op=ALU.mult), reads=[gb[2], g_rope], writes=[g_f1])
                P.op("dve", lambda e, gs=gs: e.tensor_tensor(out=f2[64:96, :], in0=banks[3][64:96, :], in1=ropeS[64:96, gs], op=ALU.mult), reads=[gb[3], g_rope], writes=[g_f2])
                P.op("dve", lambda e, gs=gs: e.tensor_tensor(out=kpe[64:96, gs], in0=f1[64:96, :], in1=f2[64:96, :], op=ALU.add), reads=[g_f1, g_f2], writes=[g_kpe])
            for t in range(NT):
                bi = t % 2
                mm_group(bi, 128, 512, [(ckvn[:, k, t * 128:(t + 1) * 128], wv[:, k].rearrange("p h e -> p (h e)")) for k in range(2)], [g_ckvn[t // 4], g_wv])
                if t % 2 == 0:
                    P.op("act", lambda e, t=t, bi=bi: e.activation(out=V_sb[:, t, :, 0:64], in_=banks[bi][:, :].rearrange("p (h e) -> p h e", e=64), func=AF.Copy),
                         reads=[gb[bi]], writes=[g_V[t]])
                else:
                    P.op("dve", lambda e, t=t, bi=bi: e.tensor_copy(out=V_sb[:, t, :, 0:64], in_=banks[bi][:, :].rearrange("p (h e) -> p h e", e=64)),
                         reads=[gb[bi]], writes=[g_V[t]])
            if debug and l == 0:
                pass

            rs2 = W.alloc("rs2", [128, 512], F32); g_rs2 = G("rs2")

            def prep_stages(h, g):
                hp = h % 2
                gs = slice(g * 512, (g + 1) * 512)
                st_ = []
                def qA():
                    mm_group(0, 96, 512, [(wq[:, k, h * 96:(h + 1) * 96], cqn[:, k, gs]) for k in range(2)], [g_wq, g_cqn[g]])
                def qB():
                    mm_group(1, 96, 512, [(wq_sw[:, k, h, :], cqn[:, k, gs]) for k in range(2)], [g_wqsw, g_cqn[g]])
                def q1():
                    P.op("dve", lambda e: e.tensor_tensor(out=f1[0:96, :], in0=banks[0][0:96, :], in1=ropeC[0:96, gs], op=ALU.mult), reads=[gb[0], g_rope], writes=[g_f1])
                    P.op("dve", lambda e: e.tensor_tensor(out=f2[0:96, :], in0=banks[1][0:96, :], in1=ropeS[0:96, gs], op=ALU.mult), reads=[gb[1], g_rope], writes=[g_f2])
                def q2():
                    P.op("pool", lambda e: e.tensor_tensor(out=f1[0:96, :], in0=f1[0:96, :], in1=f2[0:96, :], op=ALU.add), reads=[g_f1, g_f2], writes=[g_f1])
                def q3():
                    P.op("pool", lambda e: e.tensor_tensor(out=sqb[0][0:96, :], in0=f1[0:96, :], in1=f1[0:96, :], op=ALU.mult), reads=[g_f1], writes=[g_sqb[0]])
                def q4():
                    mm_group(0, 96, 512, [(ones_bf[0:96, 0:96], sqb[0][0:96, :])], [g_ones_bf, g_sqb[0]])
                def q5():
                    P.op("dve", lambda e: e.tensor_scalar(out=rs[0:96, :], in0=banks[0][0:96, :], scalar1=1.0 / 96, scalar2=EPS, op0=ALU.mult, op1=ALU.add), reads=[gb[0]], writes=[g_rs])
                def q6():
                    P.op("act", lambda e: e.activation(out=rs[0:96, :], in_=rs[0:96, :], func=AF.Ln), reads=[g_rs], writes=[g_rs])
                def q7():
                    P.op("act", lambda e: e.activation(out=rs[0:96, :], in_=rs[0:96, :], func=AF.Exp, scale=-0.5), reads=[g_rs], writes=[g_rs])
                def q8():
                    P.op("dve", lambda e: e.scalar_tensor_tensor(out=qh[hp][0:96, gs], in0=f1[0:96, :], scalar=qhg[0:96, 0:1], in1=rs[0:96, :], op0=ALU.mult, op1=ALU.mult),
                         reads=[g_f1, g_hg, g_rs], writes=[g_qh[hp]])
                def k0():
                    mm_group(2, 64, 512, [(wkn[:, k, h, :], ckvn[:, k, gs]) for k in range(2)], [g_wkn, g_ckvn[g]])
                def k1():
                    P.op("dve", lambda e: e.tensor_copy(out=f3[0:64, :], in_=banks[2][0:64, :]), reads=[gb[2]], writes=[g_f3])
                    P.op("pool", lambda e: e.tensor_copy(out=f3[64:96, :], in_=kpe[64:96, gs]), reads=[g_kpe], writes=[g_f3])
                def k2():
                    P.op("pool", lambda e: e.tensor_tensor(out=sqb[1][0:96, :], in0=f3[0:96, :], in1=f3[0:96, :], op=ALU.mult), reads=[g_f3], writes=[g_sqb[1]])
                def k3():
                    mm_group(2, 96, 512, [(ones_bf[0:96, 0:96], sqb[1][0:96, :])], [g_ones_bf, g_sqb[1]])
                def k4():
                    P.op("dve", lambda e: e.tensor_scalar(out=rs2[0:96, :], in0=banks[2][0:96, :], scalar1=1.0 / 96, scalar2=EPS, op0=ALU.mult, op1=ALU.add), reads=[gb[2]], writes=[g_rs2])
                def k5():
                    P.op("act", lambda e: e.activation(out=rs2[0:96, :], in_=rs2[0:96, :], func=AF.Ln), reads=[g_rs2], writes=[g_rs2])
                def k6():
                    P.op("act", lambda e: e.activation(out=rs2[0:96, :], in_=rs2[0:96, :], func=AF.Exp, scale=-0.5), reads=[g_rs2], writes=[g_rs2])
                def k7():
                    P.op("dve", lambda e: e.scalar_tensor_tensor(out=kh[hp][0:96, gs], in0=f3[0:96, :], scalar=khg[0:96, 0:1], in1=rs2[0:96, :], op0=ALU.mult, op1=ALU.mult),
                         reads=[g_f3, g_hg, g_rs2], writes=[g_kh[hp]])
                return [[qA], [qB], [k0], [q1], [k1], [q2], [k2], [q3], [], [k3], [k4], [q4], [q5, k5], [k6, q6], [k7, q7], [q8]]

            for g in range(NG):
                for grp_ in prep_stages(0, g):
                    for fn_ in grp_:
                        fn_()

            pending_epi = []

            def make_epilogue(h, g, ob):
                gs = slice(g * 512, (g + 1) * 512)
                def e0():
                    P.op("dve", lambda e: e.reciprocal(out=rden[64:65, :], in_=banks[ob][64:65, :]), reads=[gb[ob]], writes=[g_rden])
                def e1():
                    P.op("pe", lambda e: e.matmul(banks[1][0:64, :], lhsT=ones_f[64:65, 0:64], rhs=rden[64:65, :], start=True, stop=True), reads=[g_rden, g_ones_f], writes=[gb[1]])
                def e2():
                    P.op("dve", lambda e: e.tensor_copy(out=b_sb[:], in_=banks[1][0:64, :]), reads=[gb[1]], writes=[g_bsb])
                def e3():
                    P.op("dve", lambda e: e.tensor_tensor(out=oT[:, h, gs], in0=banks[ob][0:64, :], in1=b_sb[:], op=ALU.mult), reads=[gb[ob], g_bsb], writes=[g_oT[h]])
                return [e0, e1, e2, e3]

            cnt = 0
            for h in range(8):
                hp = h % 2
                if debug and l == 0 and h == 0:
                    P.dma("gq", dbg["d_qh"][:, :], qh[0][0:96, :], reads=[g_qh[0]])
                    P.dma("gq", dbg["d_kh"][:, :], kh[0][0:96, :], reads=[g_kh[0]])
                    P.dma("gq", dbg["d_cqn"].rearrange("p (c t) -> p c t", c=2), cqn[:], reads=g_cqn)
                for g in range(NG):
                    gs = slice(g * 512, (g + 1) * 512)
                    ob = 6 + (g % 2)
                    nxt = prep_stages(h + 1, g) if h < 7 else []

                    def qk(kt, c):
                        sbk = 3 + (c % 3)
                        P.op("pe", lambda e, sbk=sbk, kt=kt, hp=hp, gs=gs: e.matmul(banks[sbk][:, :], lhsT=kh[hp][0:96, kt * 128:(kt + 1) * 128], rhs=qh[hp][0:96, gs], start=True, stop=True),
                             reads=[g_kh[hp], g_qh[hp]], writes=[gb[sbk]])
                    qk(0, cnt)
                    qk(1, cnt + 1)
                    for kt in range(NT):
                        c = cnt + kt
                        sbk = 3 + (c % 3); pi = c % 4
                        if kt + 2 < NT:
                            qk(kt + 2, c + 2)
                        P.op("act", lambda e, sbk=sbk, pi=pi: e.activation(out=PT[pi][:], in_=banks[sbk][:, :], func=AF.Exp), reads=[gb[sbk]], writes=[g_PT[pi]])
                        P.op("pe", lambda e, ob=ob, kt=kt, pi=pi, h=h: e.matmul(banks[ob][0:65, :], lhsT=V_sb[:, kt, h, :], rhs=PT[pi][:], start=(kt == 0), stop=(kt == NT - 1)),
                             reads=[g_V[kt], g_PT[pi]], writes=[gb[ob]])
                        if pending_epi and kt in (0, 5, 7, 9):
                            pending_epi.pop(0)()
                        if nxt:
                            for fn_ in nxt.pop(0):
                                fn_()
                    cnt += NT
                    while nxt:
                        for fn_ in nxt.pop(0):
                            fn_()
                    while pending_epi:
                        pending_epi.pop(0)()
                    pending_epi = make_epilogue(h, g, ob)
            while pending_epi:
                pending_epi.pop(0)()
            if debug and l == 0:
                P.dma("gq", dbg["d_oT"].rearrange("p (c t) -> p c t", c=8), oT[:], reads=g_oT)
            P.barrier()

            X.reset(); oT2 = X.alloc("oT", [64, 8, T], BF16)
            ylT = X.alloc("ylT", [128, 8, T], BF16); g_yl = [G("yl%d" % n) for n in range(8)]
            W.reset()
            Suc = W.alloc("Suc", [128, T], F32); g_uc = G("Suc")
            Sa = [W.alloc("Sa%d" % d, [128, T], F32) for d in range(2)]; g_a = [G("Sa0"), G("Sa1")]
            Sb = [W.alloc("Sb%d" % d, [128, T], F32) for d in range(2)]; g_b = [G("Sb0"), G("Sb1")]
            Sm = [W.alloc("Sm%d" % d, [128, T], F32) for d in range(2)]; g_m = [G("Sm0"), G("Sm1")]
            Shf = W.alloc("Shf", [128, T], F32); g_hf = G("Shf")
            u_raw = [W.alloc("u_raw%d" % i, [128, T + 4], F32) for i in range(2)]; g_u = [G("u_raw0"), G("u_raw1")]
            ucb = W.alloc("ucb", [128, T], BF16); g_ucb = G("ucb")
            wu = [W.alloc("wu%d" % i, [128, 8, 128], BF16) for i in range(2)]; g_wu = [G("wu0"), G("wu1")]
            wy = [W.alloc("wy%d" % i, [128, 8, 128], BF16) for i in range(2)]; g_wy = [G("wy0"), G("wy1")]
            gw = [W.alloc("gw%d" % i, [128, 4, 128], BF16) for i in range(2)]; g_gw = [G("gw0"), G("gw1")]
            for i in range(2):
                P.op("dve", lambda e, i=i: e.memset(u_raw[i][:, 0:2], 0.0), writes=[g_u[i]])
                P.op("dve", lambda e, i=i: e.memset(u_raw[i][:, T + 2:T + 4], 0.0), writes=[g_u[i]])

            def lru_U(n):
                i = n % 2
                P.dma("gq", wu[i][:], wi[:, :, 544 + n * 128: 544 + (n + 1) * 128], writes=[g_wu[i]])
                P.dma("gq", gw[i][:], lru_gate_w[l][:, :, n].rearrange("a b d e -> d (a b) e"), writes=[g_gw[i]])
                P.dma("gq", wy[i][:], wi[:, :, 1568 + n * 128: 1568 + (n + 1) * 128], writes=[g_wy[i]])
                for g in range(NG):
                    gs = slice(g * 512, (g + 1) * 512)
                    bi = g % 2
                    mm_group(bi, 128, 512, [(wu[i][:, k, :], hT[:, k, gs]) for k in range(KC)], [g_hT[c][g] for c in range(KC)] + [g_wu[i]])
                    P.op("act", lambda e, bi=bi, g=g, i=i: e.activation(out=u_raw[i][:, 2 + g * 512: 2 + (g + 1) * 512], in_=banks[bi][:, :], func=AF.Copy), reads=[gb[bi]], writes=[g_u[i]])

            def lru_conv(n):
                i = n % 2; ur = u_raw[i]
                P.op("dve", lambda e: e.tensor_scalar(out=Suc[:], in0=ur[:, 0:T], scalar1=parB[:, n:n + 1], scalar2=parB[:, 32 + n:32 + n + 1], op0=ALU.mult, op1=ALU.add),
                     reads=[g_u[i], g_parB], writes=[g_uc])
                for tap in range(1, 4):
                    P.op("dve", lambda e, tap=tap: e.scalar_tensor_tensor(out=Suc[:], in0=ur[:, tap:tap + T], scalar=parB[:, tap * 8 + n:tap * 8 + n + 1], in1=Suc[:], op0=ALU.mult, op1=ALU.add),
                         reads=[g_u[i], g_parB, g_uc], writes=[g_uc])

            def lru_ucb():
                P.op("act", lambda e: e.activation(out=ucb[:], in_=Suc[:], func=AF.Copy), reads=[g_uc], writes=[g_ucb])

            lru_U(0)
            lru_U(1)
            lru_conv(0)
            lru_ucb()
            for n in range(8):
                i = n % 2
                for d in range(2):
                    for gate in range(2):
                        dst = Sa[d] if gate == 0 else Sb[d]; g_dst = g_a[d] if gate == 0 else g_b[d]
                        for g in range(NG):
                            gs = slice(g * 512, (g + 1) * 512)
                            bi = 2 + (g % 2)
                            mm_group(bi, 128, 512, [(gw[i][:, d * 2 + gate, :], ucb[:, gs])], [g_gw[i], g_ucb])
                            bcol = 40 + (d * 2 + gate) * 8 + n
                            P.op("act", lambda e, bi=bi, dst=dst, gs=gs, bcol=bcol: e.activation(out=dst[:, gs], in_=banks[bi][:, :], func=AF.Sigmoid, bias=parB[:, bcol:bcol + 1]),
                                 reads=[gb[bi], g_parB], writes=[g_dst])
                for d in range(2):
                    P.op("dve", lambda e, d=d: e.tensor_tensor(out=Sb[d][:], in0=Sb[d][:], in1=Suc[:], op=ALU.mult), reads=[g_b[d], g_uc], writes=[g_b[d]])
                if n + 1 < 8:
                    lru_conv(n + 1)
                for d in range(2):
                    ncol = d * 8 + n
                    P.op("act", lambda e, ncol=ncol, d=d: e.activation(out=Sm[d][:], in_=Sa[d][:], func=AF.Exp, scale=nsp2[:, ncol:ncol + 1]), reads=[g_a[d], g_nsp], writes=[g_m[d]])
                    P.op("act", lambda e, ncol=ncol, d=d: e.activation(out=Sa[d][:], in_=Sa[d][:], func=AF.Exp, scale=nsp[:, ncol:ncol + 1]), reads=[g_a[d], g_nsp], writes=[g_a[d]])
                for d in range(2):
                    P.op("act", lambda e, d=d: e.activation(out=Sm[d][:], in_=Sm[d][:], func=AF.Sqrt, scale=-1.0, bias=1.0), reads=[g_m[d]], writes=[g_m[d]])
                if n + 1 < 8:
                    lru_ucb()
                for d in range(2):
                    first = 0 if d == 0 else T - 1
                    P.op("dve", lambda e, first=first, d=d: e.memset(Sm[d][:, first:first + 1], 1.0), reads=[g_m[d]], writes=[g_m[d]])
                    P.op("dve", lambda e, d=d: e.tensor_tensor(out=Sb[d][:], in0=Sb[d][:], in1=Sm[d][:], op=ALU.mult), reads=[g_b[d], g_m[d]], writes=[g_b[d]])
                    if d == 0:
                        P.op("dve", lambda e: e.tensor_tensor_scan(out=Shf[:], data0=Sa[0][:], data1=Sb[0][:], initial=0.0, op0=ALU.mult, op1=ALU.add),
                             reads=[g_a[0], g_b[0]], writes=[g_hf])
                    else:
                        P.op("dve", lambda e: e.tensor_tensor_scan(out=Sm[1][:, ::-1], data0=Sa[1][:, ::-1], data1=Sb[1][:, ::-1], initial=0.0, op0=ALU.mult, op1=ALU.add),
                             reads=[g_a[1], g_b[1], g_m[1]], writes=[g_m[1]])
                for g in range(NG):
                    gs = slice(g * 512, (g + 1) * 512)
                    bi = 4 + (g % 2)
                    mm_group(bi, 128, 512, [(wy[i][:, k, :], hT[:, k, gs]) for k in range(KC)], [g_hT[c][g] for c in range(KC)] + [g_wy[i]])
                    P.op("act", lambda e, bi=bi, gs=gs: e.activation(out=Sm[0][:, gs], in_=banks[bi][:, :], func=AF.Gelu_apprx_tanh), reads=[gb[bi]], writes=[g_m[0]])
                if n + 2 < 8:
                    lru_U(n + 2)
                P.op("dve", lambda e: e.tensor_tensor(out=Shf[:], in0=Shf[:], in1=Sm[1][:], op=ALU.add), reads=[g_hf, g_m[1]], writes=[g_hf])
                P.op("dve", lambda e, n=n: e.tensor_tensor(out=ylT[:, n, :], in0=Shf[:], in1=Sm[0][:], op=ALU.mult), reads=[g_hf, g_m[0]], writes=[g_yl[n]])
            if debug and l == 0:
                P.dma("gq", dbg["d_ylT"].rearrange("p (c t) -> p c t", c=8), ylT[:], reads=g_yl)
            P.barrier()

            W.reset()
            zT = W.alloc("zT", [128, 8, T], BF16); g_z = [G("z%d" % j) for j in range(8)]
            wo = W.alloc("wo", [128, 8, D], BF16); g_wo = G("wo")
            cw_ = []
            for i in range(2):
                cw_.append(dict(gl=W.alloc("cgl%d" % i, [128, 8, 128], BF16), gm=W.alloc("cgm%d" % i, [128, 8, 128], BF16),
                                ol=W.alloc("col%d" % i, [128, 8, 128], BF16), om=W.alloc("com%d" % i, [64, 8, 128], BF16), g=G("cw%d" % i)))
            sg = [W.alloc("sg%d" % i, [128, 512], F32) for i in range(2)]; g_sg = [G("sg0"), G("sg1")]
            zl = W.alloc("zl", [128, 512], F32); g_zl = G("zl")
            def load_wo():
                P.dma("gq", wo[:], wview(w_out[l]), writes=[g_wo])
                for k in range(KC):
                    for hf in range(2):
                        P.op("dve", lambda e, k=k, hf=hf: e.tensor_tensor(out=wo[:, k, hf * 512:(hf + 1) * 512], in0=wo[:, k, hf * 512:(hf + 1) * 512], in1=gate_bc[0][:, hf * 512:(hf + 1) * 512], op=ALU.mult),
                             reads=[g_wo, g_gate[0]], writes=[g_wo])
            wol = wview(w_o_lru[l]); wom = w_o_mla[l].rearrange("(h p) n -> p h n", p=64)
            def load_cw(j):
                cwj = cw_[j % 2]
                P.dma("gq", cwj["gl"][:], wi[:, :, 2592 + j * 128: 2592 + (j + 1) * 128], writes=[cwj["g"]])
                P.dma("gq", cwj["ol"][:], wol[:, :, j * 128:(j + 1) * 128], writes=[cwj["g"]])
                P.dma("gq", cwj["gm"][:], wi[:, :, 3616 + j * 128: 3616 + (j + 1) * 128], writes=[cwj["g"]])
                P.dma("gq", cwj["om"][:], wom[:, :, j * 128:(j + 1) * 128], writes=[cwj["g"]])
            load_cw(0)
            for j in range(8):
                i = j % 2; cwj = cw_[i]
                if j + 1 < 8:
                    load_cw(j + 1)
                if j == 2:
                    load_wo()
                for g in range(NG):
                    gs = slice(g * 512, (g + 1) * 512)
                    hT_g = [g_hT[c][g] for c in range(KC)]
                    mm_group(0, 128, 512, [(cwj["gl"][:, k, :], hT[:, k, gs]) for k in range(KC)], hT_g + [cwj["g"]])
                    P.op("act", lambda e: e.activation(out=sg[0][:], in_=banks[0][:, :], func=AF.Sigmoid), reads=[gb[0]], writes=[g_sg[0]])
                    mm_group(1, 128, 512, [(cwj["ol"][:, k, :], ylT[:, k, gs]) for k in range(KC)], g_yl + [cwj["g"]])
                    P.op("dve", lambda e: e.tensor_tensor(out=zl[:], in0=banks[1][:, :], in1=sg[0][:], op=ALU.mult), reads=[gb[1], g_sg[0]], writes=[g_zl])
                    mm_group(2, 128, 512, [(cwj["gm"][:, k, :], hT[:, k, gs]) for k in range(KC)], hT_g + [cwj["g"]])
                    P.op("act", lambda e: e.activation(out=sg[1][:], in_=banks[2][:, :], func=AF.Sigmoid), reads=[gb[2]], writes=[g_sg[1]])
                    mm_group(3, 128, 512, [(cwj["om"][:, hh, :], oT2[:, hh, gs]) for hh in range(8)], g_oT + [cwj["g"]])
                    P.op("dve", lambda e: e.tensor_tensor(out=sg[1][:], in0=banks[3][:, :], in1=sg[1][:], op=ALU.mult), reads=[gb[3], g_sg[1]], writes=[g_sg[1]])
                    P.op("dve", lambda e, j=j, gs=gs: e.tensor_tensor(out=zT[:, j, gs], in0=zl[:], in1=sg[1][:], op=ALU.add), reads=[g_zl, g_sg[1]], writes=[g_z[j]])
            if debug and l == 0:
                P.dma("gq", dbg["d_zT"].rearrange("p (c t) -> p c t", c=8), zT[:], reads=g_z)
            P.barrier()

            X.reset(); x_sb2 = X.alloc("x_sb", [128, NT, D], F32)
            src = x_dt if l == layers[0] else xs_dt
            for t in range(NT):
                P.dma("sp", x_sb2[:, t, :], src[:, t, :], reads=([g_xscr[t]] if l != layers[0] else []), writes=[g_x[t]])
            for t in range(NT):
                for hf in range(2):
                    bi = (2 * t + hf) % 4
                    mm_group(bi, 128, 512, [(zT[:, k, t * 128:(t + 1) * 128], wo[:, k, hf * 512:(hf + 1) * 512]) for k in range(KC)], g_z + [g_wo])
                    P.op("dve", lambda e, t=t, hf=hf, bi=bi: e.tensor_tensor(out=x_sb2[:, t, hf * 512:(hf + 1) * 512], in0=banks[bi][:, :], in1=x_sb2[:, t, hf * 512:(hf + 1) * 512], op=ALU.add),
                         reads=[gb[bi], g_x[t]], writes=[g_x[t]])
            if debug and l == 0:
                for t in range(NT):
                    P.dma("sp", dbg["d_x1"].rearrange("(t p) d -> p t d", p=128)[:, t, :], x_sb2[:, t, :], reads=[g_x[t]])
            P.barrier()

            W.reset()
            is_moe = (l % 2 == 1)
            norm_phase(1, W, router_w=(moe_router[l // 2] if is_moe else None))
            if debug and is_moe:
                P.dma("sp", dbg["d_comb"][:, :], comb[:].rearrange("p a b -> p (a b)"), reads=[g_comb])
            P.barrier()

            W.reset()
            MAXH = max(HSPLIT)
            actT = W.alloc("actT", [128, MAXH, T], BF16); g_act = [G("act%d" % j) for j in range(MAXH)]
            wd = [W.alloc("wd%d" % i, [128, MAXH, D], BF16) for i in range(2)]; g_wd = [G("wd0"), G("wd1")]
            wg = [W.alloc("wg%d" % i, [128, 8, 128], BF16) for i in range(4)]; g_wg = [G("wg%d" % i) for i in range(4)]
            wu_ = [W.alloc("wup%d" % i, [128, 8, 128], BF16) for i in range(4)]; g_wup = [G("wup%d" % i) for i in range(4)]
            sgf = [W.alloc("sgf%d" % i, [128, 512], F32) for i in range(3)]; g_sgf = [G("sgf%d" % i) for i in range(3)]
            experts = range(nexp) if is_moe else [None]
            passno = 0; wcnt = 0; scnt = 0
            awb = [W.alloc("adawF%d" % i, [128, 8, 512], BF16) for i in range(2)]; g_awb = [G("adawF0"), G("adawF1")]
            unit = 0
            n_exp_ = len(list(experts))
            for ex in experts:
                if is_moe:
                    Wg = wview(moe_w_gate[l // 2, ex]); Wu = wview(moe_w_up[l // 2, ex]); Wd = moe_w_down[l // 2, ex]
                else:
                    Wg = wview(ffn_w_gate[l // 2]); Wu = wview(ffn_w_up[l // 2]); Wd = ffn_w_down[l // 2]
                Wdv = Wd.rearrange("(j p) n -> p j n", p=128)
                j0 = 0
                for nh in HSPLIT:
                    wi_ = passno % 2; passno += 1

                    def load_wd(wi_=wi_, nh=nh, j0=j0, Wdv=Wdv):
                        P.dma("gq", wd[wi_][:, 0:nh, :], Wdv[:, j0:j0 + nh, :], writes=[g_wd[wi_]])
                        for jj_ in range(nh):
                            for hf in range(2):
                                P.op("pool", lambda e, jj_=jj_, hf=hf: e.tensor_tensor(out=wd[wi_][:, jj_, hf * 512:(hf + 1) * 512], in0=wd[wi_][:, jj_, hf * 512:(hf + 1) * 512],
                                                                                     in1=gate_bc[1][:, hf * 512:(hf + 1) * 512], op=ALU.mult),
                                     reads=[g_wd[wi_], g_gate[1]], writes=[g_wd[wi_]])
                    last_pass = (ex == (list(experts)[-1])) and (j0 + nh == DFF // 128)
                    for jj in range(nh):
                        j = j0 + jj
                        if l_next is not None:
                            if unit == 0:
                                load_params(l_next)
                                ada_gate_bias(l_next, 0)
                            if 1 <= unit <= 10:
                                ada_dma(l_next, unit - 1, awb, g_awb)
                            if 2 <= unit <= 11:
                                ada_compute(l_next, unit - 2, awb, g_awb)
                            if last_pass:
                                if jj == 2:
                                    ada_gate_bias(l_next, 1)
                                    ada_dma(l_next, 10, awb, g_awb)
                                    ada_dma(l_next, 11, awb, g_awb)
                                if jj == 3:
                                    ada_compute(l_next, 10, awb, g_awb)
                                if jj == 4:
                                    ada_compute(l_next, 11, awb, g_awb)
                                    ada_final()
                        unit += 1
                        wslot = wcnt % 4; wcnt += 1
                        P.dma("gq", wg[wslot][:], Wg[:, :, j * 128:(j + 1) * 128], writes=[g_wg[wslot]])
                        P.dma("gq", wu_[wslot][:], Wu[:, :, j * 128:(j + 1) * 128], writes=[g_wup[wslot]])
                        if jj == 1:
                            load_wd()
                        for g in range(NG):
                            gs = slice(g * 512, (g + 1) * 512)
                            hT_g = [g_hT[c][g] for c in range(KC)]
                            b0 = (2 * g) % 4; b1 = b0 + 1
                            mm_group(b0, 128, 512, [(wg[wslot][:, k, :], hT[:, k, gs]) for k in range(KC)], hT_g + [g_wg[wslot]])
                            mm_group(b1, 128, 512, [(wu_[wslot][:, k, :], hT[:, k, gs]) for k in range(KC)], hT_g + [g_wup[wslot]])
                            si = scnt % 3; scnt += 1
                            P.op("act", lambda e, si=si, b0=b0: e.activation(out=sgf[si][:], in_=banks[b0][:, :], func=AF.Silu), reads=[gb[b0]], writes=[g_sgf[si]])
                            P.op("dve", lambda e, si=si, b1=b1, jj=jj, gs=gs: e.tensor_tensor(out=actT[:, jj, gs], in0=banks[b1][:, :], in1=sgf[si][:], op=ALU.mult),
                                 reads=[gb[b1], g_sgf[si]], writes=[g_act[jj]])
                    for t in range(NT):
                        for hf in range(2):
                            bi = 4 + (2 * t + hf) % 4
                            mm_group(bi, 128, 512, [(actT[:, jj, t * 128:(t + 1) * 128], wd[wi_][:, jj, hf * 512:(hf + 1) * 512]) for jj in range(nh)], g_act[0:nh] + [g_wd[wi_]])
                            if is_moe:
                                P.op("dve", lambda e, t=t, hf=hf, bi=bi, ex=ex: e.scalar_tensor_tensor(out=x_sb2[:, t, hf * 512:(hf + 1) * 512], in0=banks[bi][:, :], scalar=comb[:, t, ex:ex + 1],
                                                                                                in1=x_sb2[:, t, hf * 512:(hf + 1) * 512], op0=ALU.mult, op1=ALU.add),
                                     reads=[gb[bi], g_x[t], g_comb], writes=[g_x[t]])
                            else:
                                P.op("dve", lambda e, t=t, hf=hf, bi=bi: e.tensor_tensor(out=x_sb2[:, t, hf * 512:(hf + 1) * 512], in0=banks[bi][:, :], in1=x_sb2[:, t, hf * 512:(hf + 1) * 512], op=ALU.add),
                                     reads=[gb[bi], g_x[t]], writes=[g_x[t]])
                            if last_pass and hf == 1:
                                dst_ = out_dt if l == layers[-1] else xs_dt
                                P.dma("sp", dst_[:, t, :], x_sb2[:, t, :], reads=[g_x[t]], writes=[g_xscr[t]])
                    j0 += nh
            P.barrier()

        P.barrier()
        blk = st.enter_context(nc.Block())
        P.emit(blk)
    return nc


_W_NAMES = ["ada_w", "ada_b", "norm1_g", "norm2_g", "w_in", "q_norm_g", "kv_norm_g", "w_uq", "w_ukv", "q_head_g", "k_head_g",
            "w_o_mla", "conv_w", "conv_b", "lru_gate_w", "lru_gate_b", "lru_a_param", "w_o_lru", "w_out",
            "ffn_w_gate", "ffn_w_up", "ffn_w_down", "moe_router", "moe_w_gate", "moe_w_up", "moe_w_down"]


def kernel(**inputs):
    n = 8
    nc = build_program(L_ALL)
    shared = {k: np.ascontiguousarray(np.asarray(inputs[k], dtype=np.float32)) for k in _W_NAMES}
    x = np.asarray(inputs["x"], dtype=np.float32); c = np.asarray(inputs["c"], dtype=np.float32)
    pos = np.asarray(inputs["positions"]).astype(np.int32)
    in_maps = []
    for b in range(n):
        m = dict(shared)
        m["x"] = np.ascontiguousarray(x[b]); m["c"] = np.ascontiguousarray(c[b]); m["positions"] = np.ascontiguousarray(pos[b])
        in_maps.append(m)
    res = run_bass_kernel_spmd(nc, in_maps, core_ids=list(range(n)))
    return np.stack([np.asarray(r["out"], dtype=np.float32) for r in res.results], axis=0)
```

```python
import math
from contextlib import ExitStack
import numpy as np
import concourse.bass as bass
import concourse.mybir as mybir
from concourse.bass_utils import run_bass_kernel_spmd

F32 = mybir.dt.float32; BF16 = mybir.dt.bfloat16; I32 = mybir.dt.int32
AF = mybir.ActivationFunctionType
ALU = mybir.AluOpType
AX = mybir.AxisListType

L_ALL = 4
D = 1024; T = 2048; NT = 16; NG = 4; KC = 8
DFF = 2816; NE = 8
EPS = 1e-6
IN_COLS = 4640
HSPLIT = [6, 6, 5, 5]


class G:
    __slots__ = ("name", "w", "r")

    def __init__(self, name):
        self.name = name; self.w = None; self.r = {}


class Q:
    def __init__(self, name, is_pe=False):
        self.name = name; self.is_pe = is_pe
        self.ops = []; self.seen = {}; self.cnt = 0; self.sem = None


class Prog:
    def __init__(self, nc, stack, dma_ring=12):
        self.nc = nc
        self.q = {}
        self.semobj = {}
        for n in ("pe", "act", "dve", "pool", "sp"):
            self.q[n] = Q(n, is_pe=(n == "pe"))
        for n in ("pe", "act", "dve", "pool"):
            self.q[n].sem = stack.enter_context(nc.semaphore("s_" + n))
            self.semobj[n] = self.q[n].sem
        self.rings = {}
        for rn in ("sp", "gq"):
            sems = [stack.enter_context(nc.semaphore("d_%s%d" % (rn, i))) for i in range(dma_ring)]
            self.rings[rn] = {"sems": sems, "uses": [0] * dma_ring, "pos": 0}
            for i, s in enumerate(sems):
                self.semobj["d_%s%d" % (rn, i)] = s

    def _waits(self, q, deps):
        need = {}
        for (k, v) in deps:
            if q.is_pe and k == "pe":
                continue
            if q.seen.get(k, 0) >= v:
                continue
            if need.get(k, 0) < v:
                need[k] = v
        for k, v in need.items():
            q.seen[k] = v
            sem = self.semobj[k]
            q.ops.append(lambda e, sem=sem, v=v: e.wait_ge(sem, v))

    @staticmethod
    def _deps(reads, writes):
        deps = []
        for g in reads:
            if g.w is not None:
                deps.append(g.w)
        for g in writes:
            if g.w is not None:
                deps.append(g.w)
            for k, v in g.r.items():
                deps.append((k, v))
        return deps

    @staticmethod
    def _mark(cid, reads, writes):
        k, v = cid
        for g in reads:
            if g.r.get(k, 0) < v:
                g.r[k] = v
        for g in writes:
            g.w = cid; g.r = {}

    def op(self, qn, fn, reads=(), writes=()):
        q = self.q[qn]
        self._waits(q, self._deps(reads, writes))
        q.cnt += 1
        sem = q.sem
        q.ops.append(lambda e, fn=fn, sem=sem: fn(e).then_inc(sem, 1))
        self._mark((qn, q.cnt), reads, writes)

    def dma(self, qn, out, in_, reads=(), writes=(), **kw):
        ring = self.rings[qn]
        q = self.q["sp"] if qn == "sp" else self.q["pool"]
        pos = ring["pos"]; ring["pos"] = (pos + 1) % len(ring["sems"])
        sem = ring["sems"][pos]; uses = ring["uses"][pos]; ring["uses"][pos] += 1
        key = "d_%s%d" % (qn, pos)
        deps = self._deps(reads, writes)
        if uses > 0:
            deps.append((key, 16 * uses))
        self._waits(q, deps)
        q.ops.append(lambda e, out=out, in_=in_, sem=sem, kw=kw: e.dma_start(out=out, in_=in_, **kw).then_inc(sem, 16))
        self._mark((key, 16 * (uses + 1)), reads, writes)

    def barrier(self):
        targets = []
        for n in ("pe", "act", "dve", "pool"):
            if self.q[n].cnt > 0:
                targets.append((n, self.q[n].cnt))
        for rn, ring in self.rings.items():
            for i, u in enumerate(ring["uses"]):
                if u > 0:
                    targets.append(("d_%s%d" % (rn, i), 16 * u))
        for n in ("pe", "act", "dve", "pool", "sp"):
            self._waits(self.q[n], [t for t in targets if t[0] != n])

    def emit(self, block):
        def mk(q):
            def body(e):
                for f in q.ops:
                    f(e)
            return body
        block.sync(mk(self.q["sp"]))
        block.tensor(mk(self.q["pe"]))
        block.scalar(mk(self.q["act"]))
        block.vector(mk(self.q["dve"]))
        block.gpsimd(mk(self.q["pool"]))


class Alloc:
    BASE = 16512
    TOP = 229344

    def __init__(self, nc):
        self.nc = nc; self.n = 0

    def at(self, name, shape, dt, off):
        sz = int(np.prod(shape[1:])) * (2 if dt == BF16 else 4)
        assert off % 4 == 0
        assert self.BASE + off + sz <= self.TOP, (name, off, sz)
        self.n += 1
        return self.nc.alloc_sbuf_tensor_at("%s_%d" % (name, self.n), list(shape), dt, offset=self.BASE + off), sz


class Region:
    def __init__(self, al, start, size, name):
        self.al = al; self.start = start; self.size = size; self.cur = 0; self.name = name

    def reset(self):
        self.cur = 0

    def alloc(self, name, shape, dt):
        cur = (self.cur + 63) // 64 * 64
        t, sz = self.al.at(name, shape, dt, self.start + cur)
        assert cur + sz <= self.size, ("region overflow", self.name, name, cur, sz, self.size)
        self.cur = cur + sz
        return t


def build_program(n_layers=L_ALL, debug=False, layers=None, nexp=NE):
    nc = bass.Bass("TRN2", target_bir_lowering=False)
    L = L_ALL

    def din(name, shape, dt=F32):
        return nc.dram_tensor(name, list(shape), dt, kind="ExternalInput").ap()

    x_d = din("x", [T, D]); c_d = din("c", [D]); pos_d = din("positions", [T], I32)
    ada_w = din("ada_w", [L, D, 6 * D]); ada_b = din("ada_b", [L, 6 * D])
    norm1_g = din("norm1_g", [L, D]); norm2_g = din("norm2_g", [L, D])
    w_in = din("w_in", [L, D, IN_COLS])
    q_norm_g = din("q_norm_g", [L, 256]); kv_norm_g = din("kv_norm_g", [L, 256])
    w_uq = din("w_uq", [L, 256, 768]); w_ukv = din("w_ukv", [L, 256, 1024])
    q_head_g = din("q_head_g", [L, 96]); k_head_g = din("k_head_g", [L, 96])
    w_o_mla = din("w_o_mla", [L, 512, D])
    conv_w = din("conv_w", [L, 4, D]); conv_b = din("conv_b", [L, D])
    lru_gate_w = din("lru_gate_w", [L, 2, 2, 8, 128, 128]); lru_gate_b = din("lru_gate_b", [L, 2, 2, D])
    lru_a_param = din("lru_a_param", [L, 2, D])
    w_o_lru = din("w_o_lru", [L, D, D]); w_out = din("w_out", [L, D, D])
    ffn_w_gate = din("ffn_w_gate", [2, D, DFF]); ffn_w_up = din("ffn_w_up", [2, D, DFF]); ffn_w_down = din("ffn_w_down", [2, DFF, D])
    moe_router = din("moe_router", [2, D, NE])
    moe_w_gate = din("moe_w_gate", [2, NE, D, DFF]); moe_w_up = din("moe_w_up", [2, NE, D, DFF]); moe_w_down = din("moe_w_down", [2, NE, DFF, D])
    out_d = nc.dram_tensor("out", [T, D], F32, kind="ExternalOutput").ap()
    x_scr = nc.dram_tensor("x_scr", [T, D], F32, kind="Internal").ap()
    ropeC_d = nc.dram_tensor("ropeC", [128, T], F32, kind="Internal").ap()
    ropeS_d = nc.dram_tensor("ropeS", [128, T], F32, kind="Internal").ap()
    dbg = {}
    if debug:
        for nm, shp in (("d_hT", [128, 8 * T]), ("d_oT", [64, 8 * T]), ("d_ylT", [128, 8 * T]), ("d_zT", [128, 8 * T]),
                        ("d_x1", [T, D]), ("d_cqn", [128, 2 * T]), ("d_qh", [96, T]), ("d_kh", [96, T]), ("d_comb", [128, 128])):
            dbg[nm] = nc.dram_tensor(nm, shp, F32, kind="ExternalOutput").ap()

    x_dt = x_d.rearrange("(t p) d -> p t d", p=128)
    xs_dt = x_scr.rearrange("(t p) d -> p t d", p=128)
    out_dt = out_d.rearrange("(t p) d -> p t d", p=128)

    with ExitStack() as st:
        P = Prog(nc, st)
        al = Alloc(nc)
        PERS = Region(al, 0, 16384, "pers")
        H = Region(al, 16384, 32768, "H")
        X = Region(al, 16384 + 32768, 65536, "X")
        W = Region(al, 16384 + 32768 + 65536, Alloc.TOP - Alloc.BASE - (16384 + 32768 + 65536), "W")

        banks = [st.enter_context(nc.psum_tensor("bank%d" % i, [128, 512], F32)) for i in range(8)]
        gb = [G("bank%d" % i) for i in range(8)]

        ident = PERS.alloc("ident", [128, 128], F32); g_ident = G("ident")
        ones_bf = PERS.alloc("ones_bf", [128, 128], BF16); g_ones_bf = G("ones_bf")
        ones_f = PERS.alloc("ones_f", [128, 64], F32); g_ones_f = G("ones_f")
        cact_bf = PERS.alloc("cact_bf", [128, 8], BF16); g_cact = G("cact")
        crep = PERS.alloc("crep", [128, 8, 128], BF16); g_crep = G("crep")
        gate_bc = [PERS.alloc("gate_bc%d" % i, [128, D], F32) for i in range(2)]; g_gate = [G("gate0"), G("gate1")]
        modT = PERS.alloc("modT", [128, 32], F32); g_modT = G("modT")
        sc = [PERS.alloc("sc%d" % i, [128, 8], F32) for i in range(2)]; g_sc = [G("sc0"), G("sc1")]
        parA = PERS.alloc("parA", [128, 68], F32); g_parA = G("parA")
        parB = PERS.alloc("parB", [128, 88], F32); g_parB = G("parB")
        qhg = PERS.alloc("qhg", [128, 1], F32); khg = PERS.alloc("khg", [128, 1], F32); g_hg = G("hg")
        nsp = PERS.alloc("nsp", [128, 16], F32); g_nsp = G("nsp")
        nsp2 = PERS.alloc("nsp2", [128, 16], F32)
        ss = PERS.alloc("ss", [128, 16], F32); g_ss = G("ss")
        rstd = PERS.alloc("rstd", [128, 16], F32); g_rstd = G("rstd")
        stageA = PERS.alloc("stageA", [128, 128], F32); g_stageA = G("stageA")
        stageB = PERS.alloc("stageB", [128, 128], F32); g_stageB = G("stageB")
        lg = PERS.alloc("lg", [128, 16, 8], F32); g_lg = G("lg")
        srt = PERS.alloc("srt", [128, 16, 8], F32); g_srt = G("srt")
        comb = PERS.alloc("comb", [128, 16, 8], F32); g_comb = G("comb")
        mask = PERS.alloc("mask", [128, 16, 8], F32); g_mask = G("mask")
        den = PERS.alloc("den", [128, 16], F32); g_den = G("den")
        cf = PERS.alloc("cf", [128, 8], F32); g_cf = G("cf")
        small_i = PERS.alloc("small_i", [128, 4], I32); small_f = PERS.alloc("small_f", [128, 4], F32); g_small = G("small")

        hT = H.alloc("hT", [128, 8, T], BF16)
        g_hT = [[G("hT%d_%d" % (c, g)) for g in range(NG)] for c in range(KC)]
        x_sb = X.alloc("x_sb", [128, NT, D], F32)
        g_x = [G("x%d" % t) for t in range(NT)]
        g_xscr = [G("xscr%d" % t) for t in range(NT)]
        g_rope_d = G("rope_d")

        rr = {"act_dve": 0}

        def alt(*names):
            rr["act_dve"] += 1
            return names[rr["act_dve"] % len(names)]

        P.op("pool", lambda e: e.iota(stageA[:].bitcast(I32), pattern=[[1, 128]], base=0, channel_multiplier=-1), writes=[g_stageA])
        P.op("dve", lambda e: e.tensor_single_scalar(out=ident[:], in_=stageA[:].bitcast(I32), scalar=0, op=ALU.is_equal), reads=[g_stageA], writes=[g_ident])
        P.op("dve", lambda e: e.memset(ones_bf[:], 1.0), writes=[g_ones_bf])
        P.op("dve", lambda e: e.memset(ones_f[:], 1.0), writes=[g_ones_f])
        P.dma("sp", stageB[0:8, :], c_d.rearrange("(k p) -> k p", p=128), writes=[g_stageB])
        P.op("pe", lambda e: e.transpose(banks[7][:, 0:8], stageB[0:8, :], ident[0:8, 0:8]), reads=[g_stageB, g_ident], writes=[gb[7]])
        P.op("act", lambda e: e.activation(out=cf[:], in_=banks[7][:, 0:8], func=AF.Silu), reads=[gb[7]], writes=[g_cf])
        P.op("dve", lambda e: e.tensor_copy(out=cact_bf[:], in_=cf[:]), reads=[g_cf], writes=[g_cact])
        for k in range(8):
            P.op("dve", lambda e, k=k: e.tensor_scalar(out=crep[:, k, :], in0=ones_bf[:], scalar1=cf[:, k:k + 1], scalar2=None, op0=ALU.mult),
                 reads=[g_cf, g_ones_bf], writes=[g_crep])

        W.reset()
        pos_i = W.alloc("pos_i", [128, T], I32); pos_f = W.alloc("pos_f", [128, T], F32)
        ang = W.alloc("ang", [128, T], F32); a2 = W.alloc("a2", [128, T], F32); tq = W.alloc("tq", [128, T], F32)
        ti = W.alloc("ti", [128, T], I32); rr_t = W.alloc("rr_t", [128, T], F32); mk = W.alloc("mk", [128, T], F32)
        tabs = [W.alloc("tabS", [128, T], F32), W.alloc("tabC", [128, T], F32)]
        g_r = G("ropework")
        P.dma("sp", pos_i[:], pos_d.partition_broadcast(128), writes=[g_r])
        P.op("dve", lambda e: e.tensor_copy(out=pos_f[:], in_=pos_i[:]), reads=[g_r], writes=[g_r])
        P.op("pool", lambda e: e.iota(small_i[:, 0:1], pattern=[[1, 1]], base=0, channel_multiplier=1), writes=[g_small])
        P.op("dve", lambda e: e.tensor_single_scalar(out=small_i[:, 1:2], in_=small_i[:, 0:1], scalar=15, op=ALU.bitwise_and), reads=[g_small], writes=[g_small])
        P.op("dve", lambda e: e.tensor_single_scalar(out=small_i[:, 2:3], in_=small_i[:, 0:1], scalar=16, op=ALU.bitwise_and), reads=[g_small], writes=[g_small])
        P.op("dve", lambda e: e.tensor_copy(out=small_f[:, 1:3], in_=small_i[:, 1:3]), reads=[g_small], writes=[g_small])
        P.op("act", lambda e: e.activation(out=small_f[:, 0:1], in_=small_f[:, 1:2], func=AF.Exp, scale=-math.log(10000.0) / 16.0), reads=[g_small], writes=[g_small])
        P.op("dve", lambda e: e.tensor_scalar(out=small_f[:, 3:4], in0=small_f[:, 2:3], scalar1=2.0 / 16.0, scalar2=-1.0, op0=ALU.mult, op1=ALU.add), reads=[g_small], writes=[g_small])
        P.op("dve", lambda e: e.tensor_scalar(out=ang[:], in0=pos_f[:], scalar1=small_f[:, 0:1], scalar2=None, op0=ALU.mult), reads=[g_r, g_small], writes=[g_r])
        C1 = 6.28125; C2 = 2 * math.pi - C1
        for ti_, shift in ((0, 0.0), (1, math.pi / 2)):
            tab = tabs[ti_]
            P.op("dve", lambda e, shift=shift: e.tensor_scalar(out=a2[:], in0=ang[:], scalar1=shift, scalar2=None, op0=ALU.add), reads=[g_r], writes=[g_r])
            P.op("dve", lambda e: e.tensor_scalar(out=tq[:], in0=a2[:], scalar1=1.0 / (2 * math.pi), scalar2=None, op0=ALU.mult), reads=[g_r], writes=[g_r])
            P.op("dve", lambda e: e.tensor_copy(out=ti[:], in_=tq[:]), reads=[g_r], writes=[g_r])
            P.op("dve", lambda e: e.tensor_copy(out=tq[:], in_=ti[:]), reads=[g_r], writes=[g_r])
            P.op("dve", lambda e: e.scalar_tensor_tensor(out=rr_t[:], in0=tq[:], scalar=-C1, in1=a2[:], op0=ALU.mult, op1=ALU.add), reads=[g_r], writes=[g_r])
            P.op("dve", lambda e: e.scalar_tensor_tensor(out=a2[:], in0=tq[:], scalar=-C2, in1=rr_t[:], op0=ALU.mult, op1=ALU.add), reads=[g_r], writes=[g_r])
            P.op("dve", lambda e: e.tensor_single_scalar(out=mk[:], in_=a2[:], scalar=math.pi, op=ALU.is_gt), reads=[g_r], writes=[g_r])
            P.op("dve", lambda e: e.scalar_tensor_tensor(out=rr_t[:], in0=mk[:], scalar=-2 * math.pi, in1=a2[:], op0=ALU.mult, op1=ALU.add), reads=[g_r], writes=[g_r])
            P.op("dve", lambda e: e.tensor_single_scalar(out=mk[:], in_=rr_t[:], scalar=-math.pi, op=ALU.is_lt), reads=[g_r], writes=[g_r])
            P.op("dve", lambda e: e.scalar_tensor_tensor(out=a2[:], in0=mk[:], scalar=2 * math.pi, in1=rr_t[:], op0=ALU.mult, op1=ALU.add), reads=[g_r], writes=[g_r])
            P.op("dve", lambda e: e.tensor_scalar(out=a2[:], in0=a2[:], scalar1=-3.14159, scalar2=3.14159, op0=ALU.max, op1=ALU.min), reads=[g_r], writes=[g_r])
            P.op("act", lambda e, tab=tab: e.activation(out=tab[:], in_=a2[:], func=AF.Sin), reads=[g_r], writes=[g_r])
        P.op("dve", lambda e: e.tensor_scalar(out=tabs[0][:], in0=tabs[0][:], scalar1=small_f[:, 3:4], scalar2=None, op0=ALU.mult), reads=[g_r, g_small], writes=[g_r])
        P.op("dve", lambda e: e.memset(tabs[0][0:64, :], 0.0), reads=[g_r], writes=[g_r])
        P.op("dve", lambda e: e.memset(tabs[1][0:64, :], 1.0), reads=[g_r], writes=[g_r])
        P.dma("sp", ropeS_d[:, :], tabs[0][:], reads=[g_r], writes=[g_rope_d])
        P.dma("sp", ropeC_d[:, :], tabs[1][:], reads=[g_r], writes=[g_rope_d])

        for t in range(NT):
            P.dma("sp", x_sb[:, t, :], x_dt[:, t, :], writes=[g_x[t]])
        P.barrier()

        def wview(w2d):
            return w2d.rearrange("(k p) n -> p k n", p=128)

        def mm_group(bank_i, rows, cols, pairs, reads, n0=0):
            n = len(pairs)
            for i, (lt, rh) in enumerate(pairs):
                P.op("pe", lambda e, lt=lt, rh=rh, i=i: e.matmul(banks[bank_i][0:rows, n0:n0 + cols], lhsT=lt, rhs=rh, start=(i == 0), stop=(i == n - 1)),
                     reads=reads, writes=[gb[bank_i]])

        def rsqrt_chain(dst, src_ap, src_g, dst_g, scale, rows=128):
            P.op("dve", lambda e: e.tensor_scalar(out=dst, in0=src_ap, scalar1=scale, scalar2=EPS, op0=ALU.mult, op1=ALU.add), reads=[src_g], writes=[dst_g])
            P.op("act", lambda e: e.activation(out=dst, in_=dst, func=AF.Ln), reads=[dst_g], writes=[dst_g])
            P.op("act", lambda e: e.activation(out=dst, in_=dst, func=AF.Exp, scale=-0.5), reads=[dst_g], writes=[dst_g])

        def load_params(l):
            P.dma("sp", stageA[0:48, :], ada_b[l].rearrange("(j p) -> j p", p=128), writes=[g_stageA])
            P.dma("sp", stageA[48:56, :], norm1_g[l].rearrange("(j p) -> j p", p=128), writes=[g_stageA])
            P.dma("sp", stageA[56:64, :], norm2_g[l].rearrange("(j p) -> j p", p=128), writes=[g_stageA])
            P.dma("sp", stageA[64:66, :], q_norm_g[l].rearrange("(j p) -> j p", p=128), writes=[g_stageA])
            P.dma("sp", stageA[66:68, :], kv_norm_g[l].rearrange("(j p) -> j p", p=128), writes=[g_stageA])
            P.dma("sp", stageB[0:32, :], conv_w[l].rearrange("t (j p) -> (t j) p", p=128), writes=[g_stageB])
            P.dma("sp", stageB[32:40, :], conv_b[l].rearrange("(j p) -> j p", p=128), writes=[g_stageB])
            P.dma("sp", stageB[40:72, :], lru_gate_b[l].rearrange("a b (j p) -> (a b j) p", p=128), writes=[g_stageB])
            P.dma("sp", stageB[72:88, :], lru_a_param[l].rearrange("a (j p) -> (a j) p", p=128), writes=[g_stageB])
            P.op("pe", lambda e: e.transpose(banks[7][:, 0:68], stageA[0:68, :], ident[0:68, 0:68]), reads=[g_stageA, g_ident], writes=[gb[7]])
            P.op("dve", lambda e: e.tensor_copy(out=parA[:], in_=banks[7][:, 0:68]), reads=[gb[7]], writes=[g_parA])
            P.op("pe", lambda e: e.transpose(banks[7][:, 0:88], stageB[0:88, :], ident[0:88, 0:88]), reads=[g_stageB, g_ident], writes=[gb[7]])
            P.op("dve", lambda e: e.tensor_copy(out=parB[:], in_=banks[7][:, 0:88]), reads=[gb[7]], writes=[g_parB])
            P.dma("sp", qhg[0:96, :], q_head_g[l].rearrange("(p o) -> p o", o=1), writes=[g_hg])
            P.dma("sp", khg[0:96, :], k_head_g[l].rearrange("(p o) -> p o", o=1), writes=[g_hg])
            P.op("dve", lambda e: e.tensor_scalar(out=qhg[0:96, :], in0=qhg[0:96, :], scalar1=96.0 ** -0.5, scalar2=None, op0=ALU.mult), reads=[g_hg], writes=[g_hg])
            P.op("act", lambda e: e.activation(out=nsp[:], in_=parB[:, 72:88], func=AF.Exp), reads=[g_parB], writes=[g_nsp])
            P.op("act", lambda e: e.activation(out=nsp[:], in_=nsp[:], func=AF.Ln, bias=1.0), reads=[g_nsp], writes=[g_nsp])
            P.op("dve", lambda e: e.tensor_scalar(out=nsp[:], in0=nsp[:], scalar1=-8.0, scalar2=None, op0=ALU.mult), reads=[g_nsp], writes=[g_nsp])
            P.op("dve", lambda e: e.tensor_scalar(out=nsp2[:], in0=nsp[:], scalar1=2.0, scalar2=None, op0=ALU.mult), reads=[g_nsp], writes=[g_nsp])

        ADA_FM = {0: 0, 1: 1, 3: 2, 4: 3}

        def ada_gate_bias(l, gi):
            s_ = 2 if gi == 0 else 5
            P.dma("sp", gate_bc[gi][:], ada_b[l, s_ * D:(s_ + 1) * D].partition_broadcast(128), writes=[g_gate[gi]])

        def ada_dma(l, b, wb, g_wb):
            i = b % 2
            P.dma("gq", wb[i][:], wview(ada_w[l])[:, :, b * 512:(b + 1) * 512], writes=[g_wb[i]])

        def ada_compute(l, b, wb, g_wb):
            s_ = b // 2; half = b % 2; i = b % 2
            if s_ in ADA_FM:
                m0 = ADA_FM[s_] * 8 + half * 4
                for jj in range(4):
                    mm_group(7, 128, 1, [(wb[i][:, k, jj * 128:(jj + 1) * 128], cact_bf[:, k:k + 1]) for k in range(8)], [g_wb[i], g_cact], n0=m0 + jj)
                pc = s_ * 8 + half * 4
                P.op("dve", lambda e: e.tensor_tensor(out=modT[:, m0:m0 + 4], in0=banks[7][:, m0:m0 + 4], in1=parA[:, pc:pc + 4], op=ALU.add),
                     reads=[gb[7], g_parA], writes=[g_modT])
            else:
                gi = 0 if s_ == 2 else 1
                bi = b % 2
                mm_group(bi, 128, 512, [(crep[:, k, :], wb[i][:, k, :]) for k in range(8)], [g_wb[i], g_crep])
                P.op("dve", lambda e: e.tensor_tensor(out=gate_bc[gi][:, half * 512:(half + 1) * 512], in0=banks[bi][:, 0:512],
                                                      in1=gate_bc[gi][:, half * 512:(half + 1) * 512], op=ALU.add),
                     reads=[gb[bi], g_gate[gi]], writes=[g_gate[gi]])

        def ada_final():
            P.op("dve", lambda e: e.scalar_tensor_tensor(out=sc[0][:], in0=modT[:, 8:16], scalar=1.0, in1=parA[:, 48:56], op0=ALU.add, op1=ALU.mult), reads=[g_modT, g_parA], writes=[g_sc[0]])
            P.op("dve", lambda e: e.scalar_tensor_tensor(out=sc[1][:], in0=modT[:, 24:32], scalar=1.0, in1=parA[:, 56:64], op0=ALU.add, op1=ALU.mult), reads=[g_modT, g_parA], writes=[g_sc[1]])

        def adaln(l):
            W.reset()
            wb = [W.alloc("adaw%d" % i, [128, 8, 512], BF16) for i in range(2)]
            g_wb = [G("adaw0"), G("adaw1")]
            ada_gate_bias(l, 0); ada_gate_bias(l, 1)
            for b in range(12):
                ada_dma(l, b, wb, g_wb)
                ada_compute(l, b, wb, g_wb)
            ada_final()

        def norm_phase(which, Wr, router_w=None):
            scv = sc[which]; shc = 0 if which == 0 else 16
            junk = Wr.alloc("junk", [128, D], BF16); g_junk = G("junk")
            xn = [Wr.alloc("xn%d" % i, [128, D], F32) for i in range(8)]; g_xn = [G("xn%d" % i) for i in range(8)]
            if router_w is not None:
                h2f = Wr.alloc("h2f", [128, 8, 512], F32); g_h2f = [G("h2f%d" % c) for c in range(8)]
                wr_f = Wr.alloc("wr_f", [128, 8, NE], F32); g_wr = G("wr_f")
                w_hi = Wr.alloc("w_hi", [128, 8, NE], BF16); w_lo = Wr.alloc("w_lo", [128, 8, NE], BF16)
                h_hi = Wr.alloc("h_hi", [128, 8, 512], BF16); h_lo = Wr.alloc("h_lo", [128, 8, 512], BF16)
                g_hhi = [G("hhi%d" % c) for c in range(8)]; g_hlo = [G("hlo%d" % c) for c in range(8)]
                for k_ in range(8):
                    P.dma("sp", wr_f[:, k_, :], router_w[k_ * 128:(k_ + 1) * 128, :], writes=[g_wr])
                P.op("dve", lambda e: e.tensor_copy(out=w_hi[:], in_=wr_f[:]), reads=[g_wr], writes=[g_wr])
                P.op("dve", lambda e: e.tensor_tensor(out=w_lo[:], in0=wr_f[:], in1=w_hi[:], op=ALU.subtract), reads=[g_wr], writes=[g_wr])
            g_ssg = [G("ssg%d" % g_) for g_ in range(NG)]; g_rstdg = [G("rstdg%d" % g_) for g_ in range(NG)]

            def stats(g_):
                for tt_ in range(4):
                    t_ = 4 * g_ + tt_
                    P.op("act", lambda e, t_=t_: e.activation(out=junk[:], in_=x_sb[:, t_, :], func=AF.Square, accum_out=ss[:, t_:t_ + 1]), reads=[g_x[t_]], writes=[g_junk, g_ssg[g_]])
                rsqrt_chain(rstd[:, 4 * g_:4 * g_ + 4], ss[:, 4 * g_:4 * g_ + 4], g_ssg[g_], g_rstdg[g_], 1.0 / D)

            stats(0)
            for g in range(NG):
                if g + 1 < NG:
                    stats(g + 1)
                for tt in range(4):
                    t = 4 * g + tt; i = t % 8
                    P.op("dve", lambda e, t=t, i=i: e.tensor_scalar(out=xn[i][:], in0=x_sb[:, t, :], scalar1=rstd[:, t:t + 1], scalar2=None, op0=ALU.mult),
                         reads=[g_x[t], g_rstdg[g]], writes=[g_xn[i]])
                for c in range(KC):
                    bi = c % 4
                    for tt in range(4):
                        i = (4 * g + tt) % 8
                        P.op("pe", lambda e, bi=bi, tt=tt, i=i, c=c: e.transpose(banks[bi][:, tt * 128:(tt + 1) * 128], xn[i][:, c * 128:(c + 1) * 128], ident[:]),
                             reads=[g_xn[i], g_ident], writes=[gb[bi]])
                    P.op("act", lambda e, bi=bi, c=c, g=g: e.activation(out=hT[:, c, g * 512:(g + 1) * 512], in_=banks[bi][:, 0:512], func=AF.Identity,
                                                                         scale=scv[:, c:c + 1], bias=modT[:, shc + c:shc + c + 1]),
                         reads=[gb[bi], g_sc[which], g_modT], writes=[g_hT[c][g]])
                    if router_w is not None:
                        P.op("dve", lambda e, bi=bi, c=c: e.tensor_scalar(out=h2f[:, c, :], in0=banks[bi][:, 0:512], scalar1=scv[:, c:c + 1], scalar2=modT[:, shc + c:shc + c + 1],
                                                                          op0=ALU.mult, op1=ALU.add),
                             reads=[gb[bi], g_sc[which], g_modT, g_hT[c][g]], writes=[g_h2f[c]])
                        P.op("act", lambda e, c=c: e.activation(out=h_hi[:, c, :], in_=h2f[:, c, :], func=AF.Copy), reads=[g_h2f[c]], writes=[g_hhi[c]])
                        P.op("dve", lambda e, c=c: e.tensor_tensor(out=h_lo[:, c, :], in0=h2f[:, c, :], in1=h_hi[:, c, :], op=ALU.subtract), reads=[g_h2f[c], g_hhi[c]], writes=[g_hlo[c]])
                if router_w is not None:
                    for tt in range(4):
                        t = 4 * g + tt
                        prs = []
                        for c in range(8):
                            prs.append((h_hi[:, c, tt * 128:(tt + 1) * 128], w_hi[:, c, :]))
                            prs.append((h_lo[:, c, tt * 128:(tt + 1) * 128], w_hi[:, c, :]))
                            prs.append((h_hi[:, c, tt * 128:(tt + 1) * 128], w_lo[:, c, :]))
                        mm_group(7, 128, 8, prs, g_hhi + g_hlo + [g_wr], n0=t * 8)
            if router_w is not None:
                P.op("dve", lambda e: e.tensor_copy(out=lg[:].rearrange("p a b -> p (a b)"), in_=banks[7][:, 0:128]), reads=[gb[7]], writes=[g_lg])
                for t in range(NT):
                    P.op("dve", lambda e, t=t: e.max(out=srt[:, t, :], in_=lg[:, t, :]), reads=[g_lg], writes=[g_srt])
                P.op("dve", lambda e: e.tensor_tensor(out=mask[:], in0=lg[:], in1=srt[:, :, 1:2].to_broadcast([128, 16, 8]), op=ALU.is_ge), reads=[g_lg, g_srt], writes=[g_mask])
                P.op("dve", lambda e: e.tensor_tensor(out=comb[:], in0=lg[:], in1=srt[:, :, 0:1].to_broadcast([128, 16, 8]), op=ALU.subtract), reads=[g_lg, g_srt], writes=[g_comb])
                P.op("act", lambda e: e.activation(out=comb[:], in_=comb[:], func=AF.Exp), reads=[g_comb], writes=[g_comb])
                P.op("dve", lambda e: e.tensor_tensor(out=comb[:], in0=comb[:], in1=mask[:], op=ALU.mult), reads=[g_comb, g_mask], writes=[g_comb])
                P.op("dve", lambda e: e.tensor_reduce(out=den[:], in_=comb[:], op=ALU.add, axis=AX.X), reads=[g_comb], writes=[g_den])
                P.op("dve", lambda e: e.reciprocal(out=den[:], in_=den[:]), reads=[g_den], writes=[g_den])
                P.op("dve", lambda e: e.tensor_tensor(out=comb[:], in0=comb[:], in1=den[:].unsqueeze(2).to_broadcast([128, 16, 8]), op=ALU.mult), reads=[g_comb, g_den], writes=[g_comb])

        layers = list(range(n_layers)) if layers is None else layers
        for li_, l in enumerate(layers):
            l_next = layers[li_ + 1] if li_ + 1 < len(layers) else None
            if li_ == 0:
                load_params(l)
                adaln(l)
                P.barrier()
            W.reset()
            norm_phase(0, W)
            if debug and l == 0:
                P.dma("gq", dbg["d_hT"].rearrange("p (c t) -> p c t", c=8), hT[:], reads=[g for row in g_hT for g in row])
            P.barrier()
            hT_all = [g for row in g_hT for g in row]

            X.reset(); W.reset()
            oT = X.alloc("oT", [64, 8, T], BF16); g_oT = [G("oT%d" % h) for h in range(8)]
            ropeC = X.alloc("ropeC", [128, T], F32); ropeS = X.alloc("ropeS", [128, T], F32); g_rope = G("rope")
            kpe = X.alloc("kpe", [128, T], F32); g_kpe = G("kpe")
            qh = [X.alloc("qh0", [128, T], BF16), W.alloc("qh1", [128, T], BF16)]; g_qh = [G("qh0"), G("qh1")]
            kh = [X.alloc("kh0", [128, T], BF16), W.alloc("kh1", [128, T], BF16)]; g_kh = [G("kh0"), G("kh1")]
            cqn = W.alloc("cqn", [128, 2, T], BF16); ckvn = W.alloc("ckvn", [128, 2, T], BF16)
            g_cqn = [G("cqn%d" % g) for g in range(NG)]; g_ckvn = [G("ckvn%d" % g) for g in range(NG)]
            V_sb = W.alloc("V_sb", [128, NT, 8, 65], BF16); g_V = [G("V%d" % t) for t in range(NT)]
            PT = [W.alloc("PT%d" % i, [128, 512], BF16) for i in range(4)]; g_PT = [G("PT%d" % i) for i in range(4)]
            in01 = W.alloc("in01", [128, 8, 512], BF16); g_in01 = G("in01")
            wkr = W.alloc("wkr", [128, 8, 96], BF16); wkr_sw = W.alloc("wkr_sw", [128, 8, 96], BF16); g_wkr = G("wkr"); g_wkrsw = G("wkrsw")
            wq = W.alloc("wq", [128, 2, 768], BF16); wq_sw = W.alloc("wq_sw", [128, 2, 8, 96], BF16); g_wq = G("wq"); g_wqsw = G("wqsw")
            wkn = W.alloc("wkn", [128, 2, 8, 64], BF16); wv = W.alloc("wv", [128, 2, 8, 64], BF16); g_wkn = G("wkn"); g_wv = G("wv")
            sqb = [W.alloc("sqb%d" % i, [128, 512], BF16) for i in range(2)]; g_sqb = [G("sqb0"), G("sqb1")]
            f1 = W.alloc("f1", [128, 512], F32); f2 = W.alloc("f2", [128, 512], F32); f3 = W.alloc("f3", [128, 512], F32); rs = W.alloc("rs", [128, 512], F32)
            g_f1, g_f2, g_f3, g_rs = G("f1"), G("f2"), G("f3"), G("rs")
            rden = W.alloc("rden", [128, 512], F32); b_sb = W.alloc("b_sb", [64, 512], F32); g_rden = G("rden"); g_bsb = G("bsb")

            P.dma("sp", ropeC[:], ropeC_d[:, :], reads=[g_rope_d], writes=[g_rope])
            P.dma("sp", ropeS[:], ropeS_d[:, :], reads=[g_rope_d], writes=[g_rope])
            wi = wview(w_in[l])
            P.dma("gq", in01[:], wi[:, :, 0:512], writes=[g_in01])
            P.dma("gq", wkr[:], wi[:, :, 448:544], writes=[g_wkr])
            P.op("dve", lambda e: e.memset(wkr_sw[:], 0.0), writes=[g_wkrsw])
            P.dma("gq", wkr_sw[:, :, 64:80], wi[:, :, 528:544], writes=[g_wkrsw])
            P.dma("gq", wkr_sw[:, :, 80:96], wi[:, :, 512:528], writes=[g_wkrsw])
            wuq = w_uq[l].rearrange("(k p) n -> p k n", p=128)
            P.dma("gq", wq[:], wuq, writes=[g_wq])
            P.op("dve", lambda e: e.memset(wq_sw[:], 0.0), writes=[g_wqsw])
            wuq4 = w_uq[l].rearrange("(k p) (h e) -> p k h e", p=128, e=96)
            for k in range(2):
                P.dma("gq", wq_sw[:, k, :, 64:80], wuq4[:, k, :, 80:96], writes=[g_wqsw])
                P.dma("gq", wq_sw[:, k, :, 80:96], wuq4[:, k, :, 64:80], writes=[g_wqsw])
            wukv4 = w_ukv[l].rearrange("(k p) (h e) -> p k h e", p=128, e=128)
            for k in range(2):
                P.dma("gq", wkn[:, k], wukv4[:, k, :, 0:64], writes=[g_wkn])
                P.dma("gq", wv[:, k], wukv4[:, k, :, 64:128], writes=[g_wv])
            P.op("dve", lambda e: e.memset(V_sb[:, :, :, 64:65], 1.0), writes=g_V)

            for g in range(NG):
                gs = slice(g * 512, (g + 1) * 512)
                hT_g = [g_hT[c][g] for c in range(KC)]
                for (dst, g_dst, off, pcol) in ((cqn, g_cqn, 0, 64), (ckvn, g_ckvn, 256, 66)):
                    for ci in range(2):
                        mm_group(ci, 128, 512, [(in01[:, k, off + ci * 128: off + (ci + 1) * 128], hT[:, k, gs]) for k in range(KC)], hT_g + [g_in01])
                        P.op("act", lambda e, ci=ci: e.activation(out=sqb[ci][:], in_=banks[ci][:, :], func=AF.Square), reads=[gb[ci]], writes=[g_sqb[ci]])
                    mm_group(4, 128, 512, [(ones_bf[:], sqb[0][:]), (ones_bf[:], sqb[1][:])], [g_ones_bf] + g_sqb)
                    rsqrt_chain(rs[:], banks[4][:, :], gb[4], g_rs, 1.0 / 256)
                    for ci in range(2):
                        P.op("dve", lambda e, ci=ci, dst=dst, pcol=pcol, gs=gs: e.scalar_tensor_tensor(out=dst[:, ci, gs], in0=banks[ci][:, :], scalar=parA[:, pcol + ci:pcol + ci + 1],
                                                                                                 in1=rs[:], op0=ALU.mult, op1=ALU.mult),
                             reads=[gb[ci], g_parA, g_rs], writes=[g_dst[g]])
                mm_group(2, 96, 512, [(wkr[:, k, :], hT[:, k, gs]) for k in range(KC)], hT_g + [g_wkr])
                mm_group(3, 96, 512, [(wkr_sw[:, k, :], hT[:, k, gs]) for k in range(KC)], hT_g + [g_wkrsw])
                P.op("dve", lambda e, gs=gs: e.tensor_tensor(out=f1[64:96, :], in0=banks[2][64:96, :], in1=ropeC[64:96, gs], op=ALU.mult), reads=[gb[2], g_rope], writes=[g_f1])
                P.op("dve", lambda e, gs=gs: e.tensor_tensor(out=f2[64:96, :], in0=banks[3][64:96, :], in1=ropeS[64:96, gs], op=ALU.mult), reads=[gb[3], g_rope], writes=[g_f2])
                P.op("dve", lambda e, gs=gs: e.tensor_tensor(out=kpe[64:96, gs], in0=f1[64:96, :], in1=f2[64:96, :], op=ALU.add), reads=[g_f1, g_f2], writes=[g_kpe])
            for t in range(NT):
                bi = t % 2
                mm_group(bi, 128, 512, [(ckvn[:, k, t * 128:(t + 1) * 128], wv[:, k].rearrange("p h e -> p (h e)")) for k in range(2)], [g_ckvn[t // 4], g_wv])
                if t % 2 == 0:
                    P.op("act", lambda e, t=t, bi=bi: e.activation(out=V_sb[:, t, :, 0:64], in_=banks[bi][:, :].rearrange("p (h e) -> p h e", e=64), func=AF.Copy),
                         reads=[gb[bi]], writes=[g_V[t]])
                else:
                    P.op("dve", lambda e, t=t, bi=bi: e.tensor_copy(out=V_sb[:, t, :, 0:64], in_=banks[bi][:, :].rearrange("p (h e) -> p h e", e=64)),
                         reads=[gb[bi]], writes=[g_V[t]])
            if debug and l == 0:
                pass

            rs2 = W.alloc("rs2", [128, 512], F32); g_rs2 = G("rs2")

            def prep_stages(h, g):
                hp = h % 2
                gs = slice(g * 512, (g + 1) * 512)
                st_ = []
                def qA():
                    mm_group(0, 96, 512, [(wq[:, k, h * 96:(h + 1) * 96], cqn[:, k, gs]) for k in range(2)], [g_wq, g_cqn[g]])
                def qB():
                    mm_group(1, 96, 512, [(wq_sw[:, k, h, :], cqn[:, k, gs]) for k in range(2)], [g_wqsw, g_cqn[g]])
                def q1():
                    P.op("dve", lambda e: e.tensor_tensor(out=f1[0:96, :], in0=banks[0][0:96, :], in1=ropeC[0:96, gs], op=ALU.mult), reads=[gb[0], g_rope], writes=[g_f1])
                    P.op("dve", lambda e: e.tensor_tensor(out=f2[0:96, :], in0=banks[1][0:96, :], in1=ropeS[0:96, gs], op=ALU.mult), reads=[gb[1], g_rope], writes=[g_f2])
                def q2():
                    P.op("pool", lambda e: e.tensor_tensor(out=f1[0:96, :], in0=f1[0:96, :], in1=f2[0:96, :], op=ALU.add), reads=[g_f1, g_f2], writes=[g_f1])
                def q3():
                    P.op("pool", lambda e: e.tensor_tensor(out=sqb[0][0:96, :], in0=f1[0:96, :], in1=f1[0:96, :], op=ALU.mult), reads=[g_f1], writes=[g_sqb[0]])
                def q4():
                    mm_group(0, 96, 512, [(ones_bf[0:96, 0:96], sqb[0][0:96, :])], [g_ones_bf, g_sqb[0]])
                def q5():
                    P.op("dve", lambda e: e.tensor_scalar(out=rs[0:96, :], in0=banks[0][0:96, :], scalar1=1.0 / 96, scalar2=EPS, op0=ALU.mult, op1=ALU.add), reads=[gb[0]], writes=[g_rs])
                def q6():
                    P.op("act", lambda e: e.activation(out=rs[0:96, :], in_=rs[0:96, :], func=AF.Ln), reads=[g_rs], writes=[g_rs])
                def q7():
                    P.op("act", lambda e: e.activation(out=rs[0:96, :], in_=rs[0:96, :], func=AF.Exp, scale=-0.5), reads=[g_rs], writes=[g_rs])
                def q8():
                    P.op("dve", lambda e: e.scalar_tensor_tensor(out=qh[hp][0:96, gs], in0=f1[0:96, :], scalar=qhg[0:96, 0:1], in1=rs[0:96, :], op0=ALU.mult, op1=ALU.mult),
                         reads=[g_f1, g_hg, g_rs], writes=[g_qh[hp]])
                def k0():
                    mm_group(2, 64, 512, [(wkn[:, k, h, :], ckvn[:, k, gs]) for k in range(2)], [g_wkn, g_ckvn[g]])
                def k1():
                    P.op("dve", lambda e: e.tensor_copy(out=f3[0:64, :], in_=banks[2][0:64, :]), reads=[gb[2]], writes=[g_f3])
                    P.op("pool", lambda e: e.tensor_copy(out=f3[64:96, :], in_=kpe[64:96, gs]), reads=[g_kpe], writes=[g_f3])
                def k2():
                    P.op("pool", lambda e: e.tensor_tensor(out=sqb[1][0:96, :], in0=f3[0:96, :], in1=f3[0:96, :], op=ALU.mult), reads=[g_f3], writes=[g_sqb[1]])
                def k3():
                    mm_group(2, 96, 512, [(ones_bf[0:96, 0:96], sqb[1][0:96, :])], [g_ones_bf, g_sqb[1]])
                def k4():
                    P.op("dve", lambda e: e.tensor_scalar(out=rs2[0:96, :], in0=banks[2][0:96, :], scalar1=1.0 / 96, scalar2=EPS, op0=ALU.mult, op1=ALU.add), reads=[gb[2]], writes=[g_rs2])
                def k5():
                    P.op("act", lambda e: e.activation(out=rs2[0:96, :], in_=rs2[0:96, :], func=AF.Ln), reads=[g_rs2], writes=[g_rs2])
                def k6():
                    P.op("act", lambda e: e.activation(out=rs2[0:96, :], in_=rs2[0:96, :], func=AF.Exp, scale=-0.5), reads=[g_rs2], writes=[g_rs2])
                def k7():
                    P.op("dve", lambda e: e.scalar_tensor_tensor(out=kh[hp][0:96, gs], in0=f3[0:96, :], scalar=khg[0:96, 0:1], in1=rs2[0:96, :], op0=ALU.mult, op1=ALU.mult),
                         reads=[g_f3, g_hg, g_rs2], writes=[g_kh[hp]])
                return [[qA], [qB], [k0], [q1], [k1], [q2], [k2], [q3], [], [k3], [k4], [q4], [q5, k5], [k6, q6], [k7, q7], [q8]]

            for g in range(NG):
                for grp_ in prep_stages(0, g):
                    for fn_ in grp_:
                        fn_()

            pending_epi = []

            def make_epilogue(h, g, ob):
                gs = slice(g * 512, (g + 1) * 512)
                def e0():
                    P.op("dve", lambda e: e.reciprocal(out=rden[64:65, :], in_=banks[ob][64:65, :]), reads=[gb[ob]], writes=[g_rden])
                def e1():
                    P.op("pe", lambda e: e.matmul(banks[1][0:64, :], lhsT=ones_f[64:65, 0:64], rhs=rden[64:65, :], start=True, stop=True), reads=[g_rden, g_ones_f], writes=[gb[1]])
                def e2():
                    P.op("dve", lambda e: e.tensor_copy(out=b_sb[:], in_=banks[1][0:64, :]), reads=[gb[1]], writes=[g_bsb])
                def e3():
                    P.op("dve", lambda e: e.tensor_tensor(out=oT[:, h, gs], in0=banks[ob][0:64, :], in1=b_sb[:], op=ALU.mult), reads=[gb[ob], g_bsb], writes=[g_oT[h]])
                return [e0, e1, e2, e3]

            cnt = 0
            for h in range(8):
                hp = h % 2
                if debug and l == 0 and h == 0:
                    P.dma("gq", dbg["d_qh"][:, :], qh[0][0:96, :], reads=[g_qh[0]])
                    P.dma("gq", dbg["d_kh"][:, :], kh[0][0:96, :], reads=[g_kh[0]])
                    P.dma("gq", dbg["d_cqn"].rearrange("p (c t) -> p c t", c=2), cqn[:], reads=g_cqn)
                for g in range(NG):
                    gs = slice(g * 512, (g + 1) * 512)
                    ob = 6 + (g % 2)
                    nxt = prep_stages(h + 1, g) if h < 7 else []

                    def qk(kt, c):
                        sbk = 3 + (c % 3)
                        P.op("pe", lambda e, sbk=sbk, kt=kt, hp=hp, gs=gs: e.matmul(banks[sbk][:, :], lhsT=kh[hp][0:96, kt * 128:(kt + 1) * 128], rhs=qh[hp][0:96, gs], start=True, stop=True),
                             reads=[g_kh[hp], g_qh[hp]], writes=[gb[sbk]])
                    qk(0, cnt)
                    qk(1, cnt + 1)
                    for kt in range(NT):
                        c = cnt + kt
                        sbk = 3 + (c % 3); pi = c % 4
                        if kt + 2 < NT:
                            qk(kt + 2, c + 2)
                        P.op("act", lambda e, sbk=sbk, pi=pi: e.activation(out=PT[pi][:], in_=banks[sbk][:, :], func=AF.Exp), reads=[gb[sbk]], writes=[g_PT[pi]])
                        P.op("pe", lambda e, ob=ob, kt=kt, pi=pi, h=h: e.matmul(banks[ob][0:65, :], lhsT=V_sb[:, kt, h, :], rhs=PT[pi][:], start=(kt == 0), stop=(kt == NT - 1)),
                             reads=[g_V[kt], g_PT[pi]], writes=[gb[ob]])
                        if pending_epi and kt in (0, 5, 7, 9):
                            pending_epi.pop(0)()
                        if nxt:
                            for fn_ in nxt.pop(0):
                                fn_()
                    cnt += NT
                    while nxt:
                        for fn_ in nxt.pop(0):
                            fn_()
                    while pending_epi:
                        pending_epi.pop(0)()
                    pending_epi = make_epilogue(h, g, ob)
            while pending_epi:
                pending_epi.pop(0)()
            if debug and l == 0:
                P.dma("gq", dbg["d_oT"].rearrange("p (c t) -> p c t", c=8), oT[:], reads=g_oT)
            P.barrier()

            X.reset(); oT2 = X.alloc("oT", [64, 8, T], BF16)
            ylT = X.alloc("ylT", [128, 8, T], BF16); g_yl = [G("yl%d" % n) for n in range(8)]
            W.reset()
            Suc = W.alloc("Suc", [128, T], F32); g_uc = G("Suc")
            Sa = [W.alloc("Sa%d" % d, [128, T], F32) for d in range(2)]; g_a = [G("Sa0"), G("Sa1")]
            Sb = [W.alloc("Sb%d" % d, [128, T], F32) for d in range(2)]; g_b = [G("Sb0"), G("Sb1")]
            Sm = [W.alloc("Sm%d" % d, [128, T], F32) for d in range(2)]; g_m = [G("Sm0"), G("Sm1")]
            Shf = W.alloc("Shf", [128, T], F32); g_hf = G("Shf")
            u_raw = [W.alloc("u_raw%d" % i, [128, T + 4], F32) for i in range(2)]; g_u = [G("u_raw0"), G("u_raw1")]
            ucb = W.alloc("ucb", [128, T], BF16); g_ucb = G("ucb")
            wu = [W.alloc("wu%d" % i, [128, 8, 128], BF16) for i in range(2)]; g_wu = [G("wu0"), G("wu1")]
            wy = [W.alloc("wy%d" % i, [128, 8, 128], BF16) for i in range(2)]; g_wy = [G("wy0"), G("wy1")]
            gw = [W.alloc("gw%d" % i, [128, 4, 128], BF16) for i in range(2)]; g_gw = [G("gw0"), G("gw1")]
            for i in range(2):
                P.op("dve", lambda e, i=i: e.memset(u_raw[i][:, 0:2], 0.0), writes=[g_u[i]])
                P.op("dve", lambda e, i=i: e.memset(u_raw[i][:, T + 2:T + 4], 0.0), writes=[g_u[i]])

            def lru_U(n):
                i = n % 2
                P.dma("gq", wu[i][:], wi[:, :, 544 + n * 128: 544 + (n + 1) * 128], writes=[g_wu[i]])
                P.dma("gq", gw[i][:], lru_gate_w[l][:, :, n].rearrange("a b d e -> d (a b) e"), writes=[g_gw[i]])
                P.dma("gq", wy[i][:], wi[:, :, 1568 + n * 128: 1568 + (n + 1) * 128], writes=[g_wy[i]])
                for g in range(NG):
                    gs = slice(g * 512, (g + 1) * 512)
                    bi = g % 2
                    mm_group(bi, 128, 512, [(wu[i][:, k, :], hT[:, k, gs]) for k in range(KC)], [g_hT[c][g] for c in range(KC)] + [g_wu[i]])
                    P.op("act", lambda e, bi=bi, g=g, i=i: e.activation(out=u_raw[i][:, 2 + g * 512: 2 + (g + 1) * 512], in_=banks[bi][:, :], func=AF.Copy), reads=[gb[bi]], writes=[g_u[i]])

            def lru_conv(n):
                i = n % 2; ur = u_raw[i]
                P.op("dve", lambda e: e.tensor_scalar(out=Suc[:], in0=ur[:, 0:T], scalar1=parB[:, n:n + 1], scalar2=parB[:, 32 + n:32 + n + 1], op0=ALU.mult, op1=ALU.add),
                     reads=[g_u[i], g_parB], writes=[g_uc])
                for tap in range(1, 4):
                    P.op("dve", lambda e, tap=tap: e.scalar_tensor_tensor(out=Suc[:], in0=ur[:, tap:tap + T], scalar=parB[:, tap * 8 + n:tap * 8 + n + 1], in1=Suc[:], op0=ALU.mult, op1=ALU.add),
                         reads=[g_u[i], g_parB, g_uc], writes=[g_uc])

            def lru_ucb():
                P.op("act", lambda e: e.activation(out=ucb[:], in_=Suc[:], func=AF.Copy), reads=[g_uc], writes=[g_ucb])

            lru_U(0)
            lru_U(1)
            lru_conv(0)
            lru_ucb()
            for n in range(8):
                i = n % 2
                for d in range(2):
                    for gate in range(2):
                        dst = Sa[d] if gate == 0 else Sb[d]; g_dst = g_a[d] if gate == 0 else g_b[d]
                        for g in range(NG):
                            gs = slice(g * 512, (g + 1) * 512)
                            bi = 2 + (g % 2)
                            mm_group(bi, 128, 512, [(gw[i][:, d * 2 + gate, :], ucb[:, gs])], [g_gw[i], g_ucb])
                            bcol = 40 + (d * 2 + gate) * 8 + n
                            P.op("act", lambda e, bi=bi, dst=dst, gs=gs, bcol=bcol: e.activation(out=dst[:, gs], in_=banks[bi][:, :], func=AF.Sigmoid, bias=parB[:, bcol:bcol + 1]),
                                 reads=[gb[bi], g_parB], writes=[g_dst])
                for d in range(2):
                    P.op("dve", lambda e, d=d: e.tensor_tensor(out=Sb[d][:], in0=Sb[d][:], in1=Suc[:], op=ALU.mult), reads=[g_b[d], g_uc], writes=[g_b[d]])
                if n + 1 < 8:
                    lru_conv(n + 1)
                for d in range(2):
                    ncol = d * 8 + n
                    P.op("act", lambda e, ncol=ncol, d=d: e.activation(out=Sm[d][:], in_=Sa[d][:], func=AF.Exp, scale=nsp2[:, ncol:ncol + 1]), reads=[g_a[d], g_nsp], writes=[g_m[d]])
                    P.op("act", lambda e, ncol=ncol, d=d: e.activation(out=Sa[d][:], in_=Sa[d][:], func=AF.Exp, scale=nsp[:, ncol:ncol + 1]), reads=[g_a[d], g_nsp], writes=[g_a[d]])
                for d in range(2):
                    P.op("act", lambda e, d=d: e.activation(out=Sm[d][:], in_=Sm[d][:], func=AF.Sqrt, scale=-1.0, bias=1.0), reads=[g_m[d]], writes=[g_m[d]])
                if n + 1 < 8:
                    lru_ucb()
                for d in range(2):
                    first = 0 if d == 0 else T - 1
                    P.op("dve", lambda e, first=first, d=d: e.memset(Sm[d][:, first:first + 1], 1.0), reads=[g_m[d]], writes=[g_m[d]])
                    P.op("dve", lambda e, d=d: e.tensor_tensor(out=Sb[d][:], in0=Sb[d][:], in1=Sm[d][:], op=ALU.mult), reads=[g_b[d], g_m[d]], writes=[g_b[d]])
                    if d == 0:
                        P.op("dve", lambda e: e.tensor_tensor_scan(out=Shf[:], data0=Sa[0][:], data1=Sb[0][:], initial=0.0, op0=ALU.mult, op1=ALU.add),
                             reads=[g_a[0], g_b[0]], writes=[g_hf])
                    else:
                        P.op("dve", lambda e: e.tensor_tensor_scan(out=Sm[1][:, ::-1], data0=Sa[1][:, ::-1], data1=Sb[1][:, ::-1], initial=0.0, op0=ALU.mult, op1=ALU.add),
                             reads=[g_a[1], g_b[1], g_m[1]], writes=[g_m[1]])
                for g in range(NG):
                    gs = slice(g * 512, (g + 1) * 512)
                    bi = 4 + (g % 2)
                    mm_group(bi, 128, 512, [(wy[i][:, k, :], hT[:, k, gs]) for k in range(KC)], [g_hT[c][g] for c in range(KC)] + [g_wy[i]])
                    P.op("act", lambda e, bi=bi, gs=gs: e.activation(out=Sm[0][:, gs], in_=banks[bi][:, :], func=AF.Gelu_apprx_tanh), reads=[gb[bi]], writes=[g_m[0]])
                if n + 2 < 8:
                    lru_U(n + 2)
                P.op("dve", lambda e: e.tensor_tensor(out=Shf[:], in0=Shf[:], in1=Sm[1][:], op=ALU.add), reads=[g_hf, g_m[1]], writes=[g_hf])
                P.op("dve", lambda e, n=n: e.tensor_tensor(out=ylT[:, n, :], in0=Shf[:], in1=Sm[0][:], op=ALU.mult), reads=[g_hf, g_m[0]], writes=[g_yl[n]])
            if debug and l == 0:
                P.dma("gq", dbg["d_ylT"].rearrange("p (c t) -> p c t", c=8), ylT[:], reads=g_yl)
            P.barrier()

            W.reset()
            zT = W.alloc("zT", [128, 8, T], BF16); g_z = [G("z%d" % j) for j in range(8)]
            wo = W.alloc("wo", [128, 8, D], BF16); g_wo = G("wo")
            cw_ = []
            for i in range(2):
                cw_.append(dict(gl=W.alloc("cgl%d" % i, [128, 8, 128], BF16), gm=W.alloc("cgm%d" % i, [128, 8, 128], BF16),
                                ol=W.alloc("col%d" % i, [128, 8, 128], BF16), om=W.alloc("com%d" % i, [64, 8, 128], BF16), g=G("cw%d" % i)))
            sg = [W.alloc("sg%d" % i, [128, 512], F32) for i in range(2)]; g_sg = [G("sg0"), G("sg1")]
            zl = W.alloc("zl", [128, 512], F32); g_zl = G("zl")
            def load_wo():
                P.dma("gq", wo[:], wview(w_out[l]), writes=[g_wo])
                for k in range(KC):
                    for hf in range(2):
                        P.op("dve", lambda e, k=k, hf=hf: e.tensor_tensor(out=wo[:, k, hf * 512:(hf + 1) * 512], in0=wo[:, k, hf * 512:(hf + 1) * 512], in1=gate_bc[0][:, hf * 512:(hf + 1) * 512], op=ALU.mult),
                             reads=[g_wo, g_gate[0]], writes=[g_wo])
            wol = wview(w_o_lru[l]); wom = w_o_mla[l].rearrange("(h p) n -> p h n", p=64)
            def load_cw(j):
                cwj = cw_[j % 2]
                P.dma("gq", cwj["gl"][:], wi[:, :, 2592 + j * 128: 2592 + (j + 1) * 128], writes=[cwj["g"]])
                P.dma("gq", cwj["ol"][:], wol[:, :, j * 128:(j + 1) * 128], writes=[cwj["g"]])
                P.dma("gq", cwj["gm"][:], wi[:, :, 3616 + j * 128: 3616 + (j + 1) * 128], writes=[cwj["g"]])
                P.dma("gq", cwj["om"][:], wom[:, :, j * 128:(j + 1) * 128], writes=[cwj["g"]])
            load_cw(0)
            for j in range(8):
                i = j % 2; cwj = cw_[i]
                if j + 1 < 8:
                    load_cw(j + 1)
                if j == 2:
                    load_wo()
                for g in range(NG):
                    gs = slice(g * 512, (g + 1) * 512)
                    hT_g = [g_hT[c][g] for c in range(KC)]
                    mm_group(0, 128, 512, [(cwj["gl"][:, k, :], hT[:, k, gs]) for k in range(KC)], hT_g + [cwj["g"]])
                    P.op("act", lambda e: e.activation(out=sg[0][:], in_=banks[0][:, :], func=AF.Sigmoid), reads=[gb[0]], writes=[g_sg[0]])
                    mm_group(1, 128, 512, [(cwj["ol"][:, k, :], ylT[:, k, gs]) for k in range(KC)], g_yl + [cwj["g"]])
                    P.op("dve", lambda e: e.tensor_tensor(out=zl[:], in0=banks[1][:, :], in1=sg[0][:], op=ALU.mult), reads=[gb[1], g_sg[0]], writes=[g_zl])
                    mm_group(2, 128, 512, [(cwj["gm"][:, k, :], hT[:, k, gs]) for k in range(KC)], hT_g + [cwj["g"]])
                    P.op("act", lambda e: e.activation(out=sg[1][:], in_=banks[2][:, :], func=AF.Sigmoid), reads=[gb[2]], writes=[g_sg[1]])
                    mm_group(3, 128, 512, [(cwj["om"][:, hh, :], oT2[:, hh, gs]) for hh in range(8)], g_oT + [cwj["g"]])
                    P.op("dve", lambda e: e.tensor_tensor(out=sg[1][:], in0=banks[3][:, :], in1=sg[1][:], op=ALU.mult), reads=[gb[3], g_sg[1]], writes=[g_sg[1]])
                    P.op("dve", lambda e, j=j, gs=gs: e.tensor_tensor(out=zT[:, j, gs], in0=zl[:], in1=sg[1][:], op=ALU.add), reads=[g_zl, g_sg[1]], writes=[g_z[j]])
            if debug and l == 0:
                P.dma("gq", dbg["d_zT"].rearrange("p (c t) -> p c t", c=8), zT[:], reads=g_z)
            P.barrier()

            X.reset(); x_sb2 = X.alloc("x_sb", [128, NT, D], F32)
            src = x_dt if l == layers[0] else xs_dt
            for t in range(NT):
                P.dma("sp", x_sb2[:, t, :], src[:, t, :], reads=([g_xscr[t]] if l != layers[0] else []), writes=[g_x[t]])
            for t in range(NT):
                for hf in range(2):
                    bi = (2 * t + hf) % 4
                    mm_group(bi, 128, 512, [(zT[:, k, t * 128:(t + 1) * 128], wo[:, k, hf * 512:(hf + 1) * 512]) for k in range(KC)], g_z + [g_wo])
                    P.op("dve", lambda e, t=t, hf=hf, bi=bi: e.tensor_tensor(out=x_sb2[:, t, hf * 512:(hf + 1) * 512], in0=banks[bi][:, :], in1=x_sb2[:, t, hf * 512:(hf + 1) * 512], op=ALU.add),
                         reads=[gb[bi], g_x[t]], writes=[g_x[t]])
            if debug and l == 0:
                for t in range(NT):
                    P.dma("sp", dbg["d_x1"].rearrange("(t p) d -> p t d", p=128)[:, t, :], x_sb2[:, t, :], reads=[g_x[t]])
            P.barrier()

            W.reset()
            is_moe = (l % 2 == 1)
            norm_phase(1, W, router_w=(moe_router[l // 2] if is_moe else None))
            if debug and is_moe:
                P.dma("sp", dbg["d_comb"][:, :], comb[:].rearrange("p a b -> p (a b)"), reads=[g_comb])
            P.barrier()

            W.reset()
            MAXH = max(HSPLIT)
            actT = W.alloc("actT", [128, MAXH, T], BF16); g_act = [G("act%d" % j) for j in range(MAXH)]
            wd = [W.alloc("wd%d" % i, [128, MAXH, D], BF16) for i in range(2)]; g_wd = [G("wd0"), G("wd1")]
            wg = [W.alloc("wg%d" % i, [128, 8, 128], BF16) for i in range(4)]; g_wg = [G("wg%d" % i) for i in range(4)]
            wu_ = [W.alloc("wup%d" % i, [128, 8, 128], BF16) for i in range(4)]; g_wup = [G("wup%d" % i) for i in range(4)]
            sgf = [W.alloc("sgf%d" % i, [128, 512], F32) for i in range(3)]; g_sgf = [G("sgf%d" % i) for i in range(3)]
            experts = range(nexp) if is_moe else [None]
            passno = 0; wcnt = 0; scnt = 0
            awb = [W.alloc("adawF%d" % i, [128, 8, 512], BF16) for i in range(2)]; g_awb = [G("adawF0"), G("adawF1")]
            unit = 0
            n_exp_ = len(list(experts))
            for ex in experts:
                if is_moe:
                    Wg = wview(moe_w_gate[l // 2, ex]); Wu = wview(moe_w_up[l // 2, ex]); Wd = moe_w_down[l // 2, ex]
                else:
                    Wg = wview(ffn_w_gate[l // 2]); Wu = wview(ffn_w_up[l // 2]); Wd = ffn_w_down[l // 2]
                Wdv = Wd.rearrange("(j p) n -> p j n", p=128)
                j0 = 0
                for nh in HSPLIT:
                    wi_ = passno % 2; passno += 1
                    P.dma("gq", wd[wi_][:, 0:nh, :], Wdv[:, j0:j0 + nh, :], writes=[g_wd[wi_]])
                    for jj in range(nh):
                        for hf in range(2):
                            P.op("pool", lambda e, wi_=wi_, jj=jj, hf=hf: e.tensor_tensor(out=wd[wi_][:, jj, hf * 512:(hf + 1) * 512], in0=wd[wi_][:, jj, hf * 512:(hf + 1) * 512],
                                                                                       in1=gate_bc[1][:, hf * 512:(hf + 1) * 512], op=ALU.mult),
                                 reads=[g_wd[wi_], g_gate[1]], writes=[g_wd[wi_]])
                    last_pass = (ex == (list(experts)[-1])) and (j0 + nh == DFF // 128)
                    for jj in range(nh):
                        j = j0 + jj
                        if l_next is not None:
                            if unit == 0:
                                load_params(l_next)
                                ada_gate_bias(l_next, 0)
                            if 1 <= unit <= 10:
                                ada_dma(l_next, unit - 1, awb, g_awb)
                            if 2 <= unit <= 11:
                                ada_compute(l_next, unit - 2, awb, g_awb)
                            if last_pass:
                                if jj == 0:
                                    ada_gate_bias(l_next, 1)
                                    ada_dma(l_next, 10, awb, g_awb)
                                    ada_dma(l_next, 11, awb, g_awb)
                                if jj == 2:
                                    ada_compute(l_next, 10, awb, g_awb)
                                if jj == 3:
                                    ada_compute(l_next, 11, awb, g_awb)
                                    ada_final()
                        unit += 1
                        wslot = wcnt % 4; wcnt += 1
                        P.dma("gq", wg[wslot][:], Wg[:, :, j * 128:(j + 1) * 128], writes=[g_wg[wslot]])
                        P.dma("gq", wu_[wslot][:], Wu[:, :, j * 128:(j + 1) * 128], writes=[g_wup[wslot]])
                        for g in range(NG):
                            gs = slice(g * 512, (g + 1) * 512)
                            hT_g = [g_hT[c][g] for c in range(KC)]
                            b0 = (2 * g) % 4; b1 = b0 + 1
                            mm_group(b0, 128, 512, [(wg[wslot][:, k, :], hT[:, k, gs]) for k in range(KC)], hT_g + [g_wg[wslot]])
                            mm_group(b1, 128, 512, [(wu_[wslot][:, k, :], hT[:, k, gs]) for k in range(KC)], hT_g + [g_wup[wslot]])
                            si = scnt % 3; scnt += 1
                            P.op("act", lambda e, si=si, b0=b0: e.activation(out=sgf[si][:], in_=banks[b0][:, :], func=AF.Silu), reads=[gb[b0]], writes=[g_sgf[si]])
                            P.op("dve", lambda e, si=si, b1=b1, jj=jj, gs=gs: e.tensor_tensor(out=actT[:, jj, gs], in0=banks[b1][:, :], in1=sgf[si][:], op=ALU.mult),
                                 reads=[gb[b1], g_sgf[si]], writes=[g_act[jj]])
                    for t in range(NT):
                        for hf in range(2):
                            bi = 4 + (2 * t + hf) % 4
                            mm_group(bi, 128, 512, [(actT[:, jj, t * 128:(t + 1) * 128], wd[wi_][:, jj, hf * 512:(hf + 1) * 512]) for jj in range(nh)], g_act[0:nh] + [g_wd[wi_]])
                            if is_moe:
                                P.op("dve", lambda e, t=t, hf=hf, bi=bi, ex=ex: e.scalar_tensor_tensor(out=x_sb2[:, t, hf * 512:(hf + 1) * 512], in0=banks[bi][:, :], scalar=comb[:, t, ex:ex + 1],
                                                                                                in1=x_sb2[:, t, hf * 512:(hf + 1) * 512], op0=ALU.mult, op1=ALU.add),
                                     reads=[gb[bi], g_x[t], g_comb], writes=[g_x[t]])
                            else:
                                P.op("dve", lambda e, t=t, hf=hf, bi=bi: e.tensor_tensor(out=x_sb2[:, t, hf * 512:(hf + 1) * 512], in0=banks[bi][:, :], in1=x_sb2[:, t, hf * 512:(hf + 1) * 512], op=ALU.add),
                                     reads=[gb[bi], g_x[t]], writes=[g_x[t]])
                            if last_pass and hf == 1:
                                dst_ = out_dt if l == layers[-1] else xs_dt
                                P.dma("sp", dst_[:, t, :], x_sb2[:, t, :], reads=[g_x[t]], writes=[g_xscr[t]])
                    j0 += nh
            P.barrier()

        P.barrier()
        blk = st.enter_context(nc.Block())
        P.emit(blk)
    return nc


_W_NAMES = ["ada_w", "ada_b", "norm1_g", "norm2_g", "w_in", "q_norm_g", "kv_norm_g", "w_uq", "w_ukv", "q_head_g", "k_head_g",
            "w_o_mla", "conv_w", "conv_b", "lru_gate_w", "lru_gate_b", "lru_a_param", "w_o_lru", "w_out",
            "ffn_w_gate", "ffn_w_up", "ffn_w_down", "moe_router", "moe_w_gate", "moe_w_up", "moe_w_down"]


def kernel(**inputs):
    n = 8
    nc = build_program(L_ALL)
    shared = {k: np.ascontiguousarray(np.asarray(inputs[k], dtype=np.float32)) for k in _W_NAMES}
    x = np.asarray(inputs["x"], dtype=np.float32); c = np.asarray(inputs["c"], dtype=np.float32)
    pos = np.asarray(inputs["positions"]).astype(np.int32)
    in_maps = []
    for b in range(n):
        m = dict(shared)
        m["x"] = np.ascontiguousarray(x[b]); m["c"] = np.ascontiguousarray(c[b]); m["positions"] = np.ascontiguousarray(pos[b])
        in_maps.append(m)
    res = run_bass_kernel_spmd(nc, in_maps, core_ids=list(range(n)))
    return np.stack([np.asarray(r["out"], dtype=np.float32) for r in res.results], axis=0)
```
